# Optimizing a Trainium2 kernel written in Bass

```python
import math
import jax
import jax.numpy as jnp
from jax import lax
import numpy as np

D_MODEL = 2048
BATCH = 32
SEQ = 256
DEPTH = 1
DEC_BATCH = 2
DEC_SEQ = 1024
PAST_LEN = 512

GRID_W = 64
NA_HEADS = 16
NA_HEAD_DIM = 128
NA_WIDTH = NA_HEADS * NA_HEAD_DIM
NA_WIN_R = 8
NA_WIN_C = 16
DENSE_KEYS_LIMIT = 2048
Q_BLOCK = 128
SSD_HEADS = 32
SSD_HEAD_DIM = 64
SSD_WIDTH = SSD_HEADS * SSD_HEAD_DIM
SSD_GROUPS = 4
SSD_STATE = 128
SSD_CONV = 5
SSD_CHUNK = 128
SSD_CONV_CH = SSD_WIDTH + 2 * SSD_GROUPS * SSD_STATE
N_EXPERTS = 32
TOP_K = 4
D_EXPERT = 2048
SWIGLU_LIMIT = 7.0
SWIGLU_ALPHA = 1.702
DN_ALPHA = (2.0 * DEPTH) ** 0.25
DN_BETA = (8.0 * DEPTH) ** -0.25
LN_EPS = 1e-5
IN_SPLITS = (NA_WIDTH, NA_WIDTH, NA_WIDTH, SSD_WIDTH, SSD_CONV_CH, 2 * SSD_HEADS, D_MODEL, D_MODEL)
IN_COLS = 3 * NA_WIDTH + SSD_WIDTH + SSD_CONV_CH + 2 * SSD_HEADS + 2 * D_MODEL

kernel_name = 'hybrid_ssd_natten_moe_prefix_dit_step'

F32 = jnp.float32


def _split_cols(t, sizes):
    idx, acc = [], 0
    for s in sizes[:-1]:
        acc += s
        idx.append(acc)
    return jnp.split(t, idx, axis=-1)


def _layer_norm(x, g=None, b=None):
    xf = x.astype(F32)
    mu = jnp.mean(xf, axis=-1, keepdims=True)
    var = jnp.mean(jnp.square(xf - mu), axis=-1, keepdims=True)
    y = (xf - mu) * lax.rsqrt(var + LN_EPS)
    if g is not None:
        y = y * g.astype(F32) + b.astype(F32)
    return y.astype(x.dtype)


def _rms_norm(x, g):
    xf = x.astype(F32)
    y = xf * lax.rsqrt(jnp.mean(jnp.square(xf), axis=-1, keepdims=True) + LN_EPS)
    return (y * g.astype(F32)).astype(x.dtype)


def _modulation(cvec, w_mod, b_mod):
    mods = jax.nn.silu(cvec) @ w_mod + b_mod
    parts = jnp.split(mods, 6, axis=-1)
    if cvec.ndim == 2:
        parts = [p[:, None, :] for p in parts]
    return parts


def _modulate(x, shift, scale):
    return _layer_norm(x) * (1.0 + scale) + shift


def _project_mixer_inputs(h, w_in):
    b, L, _ = h.shape
    q, k, v, z, xbc, dt_raw, g_ssd, g_na = _split_cols(h @ w_in, IN_SPLITS)
    heads = lambda t: t.reshape(b, L, NA_HEADS, NA_HEAD_DIM)
    return heads(q), heads(k), heads(v), z, xbc, dt_raw, g_ssd, g_na


def _dwconv_centred(u, w, bias):
    ch, kw = u.shape[-1], w.shape[0]
    y = lax.conv_general_dilated(u, w[:, None, :].astype(u.dtype), window_strides=(1,),
                                 padding=[(kw // 2, kw // 2)],
                                 dimension_numbers=('NWC', 'WIO', 'NWC'),
                                 feature_group_count=ch)
    return y + bias


def _segsum_exp(cs):
    t = cs.shape[-1]
    diff = cs[..., :, None] - cs[..., None, :]
    mask = jnp.tril(jnp.ones((t, t), dtype=bool))
    return jnp.exp(jnp.where(mask, diff, -jnp.inf))


def _ssd_scan(x, dt, a, bm, cm, h0):
    b, L, H, P = x.shape
    G, N = bm.shape[2], bm.shape[3]
    R, Q = H // G, SSD_CHUNK
    nc = L // Q
    xdt = (x.astype(F32) * dt[..., None]).reshape(b, nc, Q, G, R, P)
    a_cs = jnp.cumsum((dt * a).reshape(b, nc, Q, G, R).transpose(0, 3, 4, 1, 2), axis=-1)
    bc = bm.astype(F32).reshape(b, nc, Q, G, N)
    cc = cm.astype(F32).reshape(b, nc, Q, G, N)
    l_intra = _segsum_exp(a_cs)
    cb = jnp.einsum('bcign,bcjgn->bgcij', cc, bc)
    y_diag = jnp.einsum('bgcij,bgrcij,bcjgrp->bcigrp', cb, l_intra, xdt)
    decay_to_end = jnp.exp(a_cs[..., -1:] - a_cs)
    chunk_states = jnp.einsum('bcjgn,bgrcj,bcjgrp->bcgrpn', bc, decay_to_end, xdt)
    h_init = h0.astype(F32).reshape(b, 1, G, R, P, N)
    chunk_states = jnp.concatenate([h_init, chunk_states], axis=1)
    chunk_cs = jnp.cumsum(jnp.pad(a_cs[..., -1], ((0, 0), (0, 0), (0, 0), (1, 0))), axis=-1)
    l_inter = _segsum_exp(chunk_cs)
    states = jnp.einsum('bgrzc,bcgrpn->bzgrpn', l_inter, chunk_states)
    y_off = jnp.einsum('bcign,bcgrpn,bgrci->bcigrp', cc, states[:, :-1], jnp.exp(a_cs))
    y = (y_diag + y_off).reshape(b, L, H, P)
    return y.astype(x.dtype), states[:, -1].reshape(b, H, P, N).astype(x.dtype)


def _ssd_branch(z, xbc, dt_raw, conv_w, conv_b, dt_bias, a_log, d_skip, norm_w, h0_fwd, h0_bwd):
    b, L, _ = z.shape
    xbc = jax.nn.silu(_dwconv_centred(xbc, conv_w, conv_b))
    xs, bm, cm = _split_cols(xbc, (SSD_WIDTH, SSD_GROUPS * SSD_STATE, SSD_GROUPS * SSD_STATE))
    xs = xs.reshape(b, L, SSD_HEADS, SSD_HEAD_DIM)
    bm = bm.reshape(b, L, SSD_GROUPS, SSD_STATE)
    cm = cm.reshape(b, L, SSD_GROUPS, SSD_STATE)
    dt = jax.nn.softplus(dt_raw.astype(F32).reshape(b, L, 2, SSD_HEADS) + dt_bias.astype(F32))
    a = -jnp.exp(a_log.astype(F32))
    flip = lambda t: jnp.flip(t, axis=1)
    y_f, h_f = _ssd_scan(xs, dt[:, :, 0], a[0], bm, cm, h0_fwd)
    y_b, h_b = _ssd_scan(flip(xs), flip(dt[:, :, 1]), a[1], flip(bm), flip(cm), h0_bwd)
    y = y_f + flip(y_b) + d_skip[:, None] * xs
    y = _rms_norm(y.reshape(b, L, SSD_WIDTH) * jax.nn.silu(z), norm_w)
    return y, h_f, h_b


def _softmax_attend(q, k, v):
    s = jnp.einsum('bqhd,bkhd->bhqk', q, k).astype(F32) * (NA_HEAD_DIM ** -0.5)
    p = jax.nn.softmax(s, axis=-1).astype(v.dtype)
    return jnp.einsum('bhqk,bkhd->bqhd', p, v)


def _context_attention(q, k, v):
    b, S, h, d = q.shape
    if k.shape[1] < DENSE_KEYS_LIMIT:
        return _softmax_attend(q, k, v)
    qb = jnp.moveaxis(q.reshape(b, S // Q_BLOCK, Q_BLOCK, h, d), 1, 0)
    ob = lax.map(lambda blk: _softmax_attend(blk, k, v), qb)
    return jnp.moveaxis(ob, 0, 1).reshape(b, S, h, d)


def _neighbourhood_attention(q, k, v, k_ctx, v_ctx, rpb):
    b, L, h, d = q.shape
    rows = L // GRID_W
    kr = min(NA_WIN_R, rows)
    scale = NA_HEAD_DIM ** -0.5
    qg = q.reshape(b, rows, GRID_W, h, d)
    kg = k.reshape(b, rows, GRID_W, h, d)
    vg = v.reshape(b, rows, GRID_W, h, d)
    col = jnp.arange(GRID_W)
    c0 = jnp.clip(col - NA_WIN_C // 2, 0, GRID_W - NA_WIN_C)
    col_mask = (col[None, :] >= c0[:, None]) & (col[None, :] < c0[:, None] + NA_WIN_C)
    dc_idx = jnp.clip(col[None, :] - col[:, None] + NA_WIN_C - 1, 0, 2 * NA_WIN_C - 2)

    def row_block(r):
        r0 = jnp.clip(r - kr // 2, 0, rows - kr)
        q_r = lax.dynamic_index_in_dim(qg, r, axis=1, keepdims=False)
        k_band = lax.dynamic_slice_in_dim(kg, r0, kr, axis=1)
        v_band = lax.dynamic_slice_in_dim(vg, r0, kr, axis=1)
        dr_idx = r0 + jnp.arange(kr) - r + NA_WIN_R - 1
        bias = rpb[:, dr_idx[None, :, None], dc_idx[:, None, :]].astype(F32)
        s_loc = jnp.einsum('bqhd,bkchd->bhqkc', q_r, k_band).astype(F32) * scale + bias
        s_loc = jnp.where(col_mask[:, None, :], s_loc, -jnp.inf).reshape(b, h, GRID_W, kr * GRID_W)
        s_ctx = jnp.einsum('bqhd,bkhd->bhqk', q_r, k_ctx).astype(F32) * scale
        p = jax.nn.softmax(jnp.concatenate([s_loc, s_ctx], axis=-1), axis=-1).astype(v.dtype)
        p_loc = p[..., :kr * GRID_W].reshape(b, h, GRID_W, kr, GRID_W)
        p_ctx = p[..., kr * GRID_W:]
        o = jnp.einsum('bhqkc,bkchd->bqhd', p_loc, v_band) + jnp.einsum('bhqk,bkhd->bqhd', p_ctx, v_ctx)
        return o.astype(q.dtype)

    out = lax.map(row_block, jnp.arange(rows))
    return jnp.moveaxis(out, 0, 1).reshape(b, L, h, d)


def _merge_branches(y_na, y_ssd, g_na, g_ssd, w_na_out, w_ssd_out, w_o):
    b, L = y_na.shape[:2]
    y = (jax.nn.sigmoid(g_na) * (y_na.reshape(b, L, NA_WIDTH) @ w_na_out)
         + jax.nn.sigmoid(g_ssd) * (y_ssd @ w_ssd_out))
    return y @ w_o


def _moe(x, w_router, b_router, w_gate_up, b_gate_up, w_down, b_down):
    b, L, d = x.shape
    xf = x.reshape(b * L, d)
    logits = xf.astype(F32) @ w_router.astype(F32) + b_router.astype(F32)
    top_vals, top_idx = lax.top_k(logits, TOP_K)
    top_w = jax.nn.softmax(top_vals, axis=-1)
    combine = jnp.einsum('tk,tke->te', top_w, jax.nn.one_hot(top_idx, N_EXPERTS, dtype=F32)).astype(x.dtype)
    out = jnp.zeros_like(xf)
    for e in range(N_EXPERTS):
        gu = xf @ w_gate_up[e] + b_gate_up[e]
        gate = jnp.minimum(gu[:, :D_EXPERT], SWIGLU_LIMIT)
        up = jnp.clip(gu[:, D_EXPERT:], -SWIGLU_LIMIT, SWIGLU_LIMIT)
        hid = (up + 1.0) * gate * jax.nn.sigmoid(SWIGLU_ALPHA * gate)
        out = out + combine[:, e:e + 1] * (hid @ w_down[e] + b_down[e])
    return out.reshape(b, L, d)


def setup_inputs(seed: int = 0) -> dict:
    key = jax.random.key(seed)
    ks = jax.random.split(key, 32)
    d = D_MODEL

    def nrm(k, shape, scale):
        return jax.random.normal(k, shape, F32) * scale

    dt0 = jnp.exp(jax.random.uniform(ks[10], (DEPTH, 2, SSD_HEADS), F32, math.log(1e-3), math.log(1e-1)))
    return {
        'x_prompt': nrm(ks[0], (BATCH, SEQ, d), 1.0),
        'x_sample': nrm(ks[1], (DEC_BATCH, DEC_SEQ, d), 1.0),
        'cache_na_k': nrm(ks[2], (DEC_BATCH, DEPTH, PAST_LEN, NA_HEADS, NA_HEAD_DIM), 1.0),
        'cache_na_v': nrm(ks[3], (DEC_BATCH, DEPTH, PAST_LEN, NA_HEADS, NA_HEAD_DIM), 1.0),
        'state_ssd_fwd': nrm(ks[4], (DEC_BATCH, DEPTH, SSD_HEADS, SSD_HEAD_DIM, SSD_STATE), 0.1),
        'state_ssd_bwd': nrm(ks[5], (DEC_BATCH, DEPTH, SSD_HEADS, SSD_HEAD_DIM, SSD_STATE), 0.1),
        'c': nrm(ks[6], (DEC_BATCH, d), 1.0),
        'c_ctx': nrm(ks[7], (d,), 1.0),
        'w_mod': nrm(ks[8], (DEPTH, d, 6 * d), 0.5 * d ** -0.5),
        'b_mod': nrm(ks[9], (DEPTH, 6 * d), 0.02),
        'w_in': nrm(ks[11], (DEPTH, d, IN_COLS), d ** -0.5),
        'ssd_conv_w': nrm(ks[12], (DEPTH, SSD_CONV, SSD_CONV_CH), SSD_CONV ** -0.5),
        'ssd_conv_b': nrm(ks[13], (DEPTH, SSD_CONV_CH), 0.02),
        'ssd_dt_bias': dt0 + jnp.log(-jnp.expm1(-dt0)),
        'ssd_a_log': jnp.log(jax.random.uniform(ks[14], (DEPTH, 2, SSD_HEADS), F32, 1.0, 16.0)),
        'ssd_d': 1.0 + nrm(ks[15], (DEPTH, SSD_HEADS), 0.1),
        'ssd_norm_w': 1.0 + nrm(ks[16], (DEPTH, SSD_WIDTH), 0.1),
        'na_rpb': nrm(ks[17], (DEPTH, NA_HEADS, 2 * NA_WIN_R - 1, 2 * NA_WIN_C - 1), 0.1),
        'w_ssd_out': nrm(ks[18], (DEPTH, SSD_WIDTH, d), SSD_WIDTH ** -0.5),
        'w_na_out': nrm(ks[19], (DEPTH, NA_WIDTH, d), NA_WIDTH ** -0.5),
        'w_o': nrm(ks[20], (DEPTH, d, d), DN_BETA * d ** -0.5),
        'ln1_g': 1.0 + nrm(ks[21], (DEPTH, d), 0.1),
        'ln1_b': nrm(ks[22], (DEPTH, d), 0.02),
        'ln2_g': 1.0 + nrm(ks[23], (DEPTH, d), 0.1),
        'ln2_b': nrm(ks[24], (DEPTH, d), 0.02),
        'w_router': nrm(ks[25], (DEPTH, d, N_EXPERTS), d ** -0.5),
        'b_router': nrm(ks[26], (DEPTH, N_EXPERTS), 0.01),
        'w_gate_up': nrm(ks[27], (DEPTH, N_EXPERTS, d, 2 * D_EXPERT), d ** -0.5),
        'b_gate_up': nrm(ks[28], (DEPTH, N_EXPERTS, 2 * D_EXPERT), 0.02),
        'w_down': nrm(ks[29], (DEPTH, N_EXPERTS, D_EXPERT, d), DN_BETA * D_EXPERT ** -0.5),
        'b_down': nrm(ks[30], (DEPTH, N_EXPERTS, d), 0.02),
    }


def reference(x_prompt, x_sample, cache_na_k, cache_na_v, state_ssd_fwd, state_ssd_bwd, c, c_ctx,
              w_mod, b_mod, w_in, ssd_conv_w, ssd_conv_b, ssd_dt_bias, ssd_a_log, ssd_d, ssd_norm_w,
              na_rpb, w_ssd_out, w_na_out, w_o, ln1_g, ln1_b, ln2_g, ln2_b,
              w_router, b_router, w_gate_up, b_gate_up, w_down, b_down):
    xp, xs = x_prompt, x_sample
    new_k, new_v, new_hf, new_hb = [], [], [], []
    for l in range(DEPTH):
        m = _modulation(c_ctx, w_mod[l], b_mod[l])
        h = _modulate(xp, m[0], m[1])
        q, k, v, z, xbc, dt_raw, g_ssd, g_na = _project_mixer_inputs(h, w_in[l])
        y_na = _context_attention(q, k, v)
        bp = xp.shape[0]
        zero_state = jnp.zeros((bp, SSD_HEADS, SSD_HEAD_DIM, SSD_STATE), xp.dtype)
        y_ssd, h_f, h_b = _ssd_branch(z, xbc, dt_raw, ssd_conv_w[l], ssd_conv_b[l], ssd_dt_bias[l],
                                      ssd_a_log[l], ssd_d[l], ssd_norm_w[l], zero_state, zero_state)
        mix = _merge_branches(y_na, y_ssd, g_na, g_ssd, w_na_out[l], w_ssd_out[l], w_o[l])
        xp = _layer_norm(DN_ALPHA * xp + m[2] * mix, ln1_g[l], ln1_b[l])
        ffn = _moe(_modulate(xp, m[3], m[4]), w_router[l], b_router[l], w_gate_up[l], b_gate_up[l],
                   w_down[l], b_down[l])
        xp = _layer_norm(DN_ALPHA * xp + m[5] * ffn, ln2_g[l], ln2_b[l])
        new_k.append(k)
        new_v.append(v)
        new_hf.append(h_f)
        new_hb.append(h_b)

        m = _modulation(c, w_mod[l], b_mod[l])
        h = _modulate(xs, m[0], m[1])
        q, k, v, z, xbc, dt_raw, g_ssd, g_na = _project_mixer_inputs(h, w_in[l])
        y_na = _neighbourhood_attention(q, k, v, cache_na_k[:, l], cache_na_v[:, l], na_rpb[l])
        y_ssd, _, _ = _ssd_branch(z, xbc, dt_raw, ssd_conv_w[l], ssd_conv_b[l], ssd_dt_bias[l],
                                  ssd_a_log[l], ssd_d[l], ssd_norm_w[l],
                                  state_ssd_fwd[:, l], state_ssd_bwd[:, l])
        mix = _merge_branches(y_na, y_ssd, g_na, g_ssd, w_na_out[l], w_ssd_out[l], w_o[l])
        xs = _layer_norm(DN_ALPHA * xs + m[2] * mix, ln1_g[l], ln1_b[l])
        ffn = _moe(_modulate(xs, m[3], m[4]), w_router[l], b_router[l], w_gate_up[l], b_gate_up[l],
                   w_down[l], b_down[l])
        xs = _layer_norm(DN_ALPHA * xs + m[5] * ffn, ln2_g[l], ln2_b[l])

    new_na_k = jnp.stack(new_k, axis=1)
    new_na_v = jnp.stack(new_v, axis=1)
    new_ssd_fwd = jnp.stack(new_hf, axis=1)
    new_ssd_bwd = jnp.stack(new_hb, axis=1)
    return (xp, xs, new_na_k, new_na_v, new_ssd_fwd, new_ssd_bwd)
```

```python
import contextlib
import numpy as np
import concourse.bass as bass
import concourse.mybir as mybir
from concourse.bass_utils import run_bass_kernel_spmd

F32 = mybir.dt.float32
BF16 = mybir.dt.bfloat16
AF = mybir.ActivationFunctionType
ALU = mybir.AluOpType
AX = mybir.AxisListType

D = 2048
NCORES = 8
LN_EPS = 1e-5
DN_ALPHA = 2.0 ** 0.25
IN_COLS = 15424
C_Q, C_K, C_V, C_Z, C_XBC, C_DT, C_GS, C_GN = 0, 2048, 4096, 6144, 8192, 11264, 11328, 13376
NEG = -1.0e30


class Buf:
    __slots__ = ("w", "r", "sem")

    def __init__(self):
        self.w = None
        self.r = []
        self.sem = None


class Eng:
    def __init__(self, h, sem, name):
        self.h, self.sem, self.name = h, sem, name
        self.cnt = 0
        self.seen = {}


class K:
    def __init__(self, nc, es):
        self.nc, self.es = nc, es
        self.sems = []
        mk = lambda n: es.enter_context(nc.semaphore(n))
        self.pe = Eng(nc.tensor, mk("s_pe"), "pe")
        self.dve = Eng(nc.vector, mk("s_dve"), "dve")
        self.act = Eng(nc.scalar, mk("s_act"), "act")
        self.pool = Eng(nc.gpsimd, mk("s_pool"), "pool")
        self.sp = Eng(nc.sync, mk("s_sp"), "sp")
        self.engs = [self.pe, self.dve, self.act, self.pool, self.sp]
        self.dma_sems = []
        self.free_sems = []
        self.nsem = 0
        self.ps = []
        self.psi = 0

    def _waits(self, eng, reads, writes):
        evs = []
        for b in reads:
            if b.w is not None:
                evs.append(b.w)
        for b in writes:
            if b.w is not None:
                evs.append(b.w)
            evs.extend(b.r)
        need = {}
        for sem, val in evs:
            k = id(sem)
            if eng.seen.get(k, 0) >= val:
                continue
            if k not in need or need[k][1] < val:
                need[k] = (sem, val)
        for k, (sem, val) in need.items():
            if sem is eng.sem and eng is self.pe:
                continue
            eng.h.wait_ge(sem, val)
            eng.seen[k] = val

    def op(self, eng, fn, reads=(), writes=()):
        self._waits(eng, reads, writes)
        ins = fn()
        eng.cnt += 1
        ins.then_inc(eng.sem, 1)
        ev = (eng.sem, eng.cnt)
        eng.seen[id(eng.sem)] = max(eng.seen.get(id(eng.sem), 0), 0)
        for b in reads:
            b.r.append(ev)
        for b in writes:
            b.w = ev
            b.r = []
        return ev

    def dma(self, eng, out, in_, reads=(), writes=(), **kw):
        self._waits(eng, reads, writes)
        tgt = writes[0] if writes else reads[0]
        if tgt.sem is None:
            if self.free_sems:
                tgt.sem = self.free_sems.pop()
            else:
                self.nsem += 1
                tgt.sem = [self.es.enter_context(self.nc.semaphore("d%d" % self.nsem)), 0]
            self.dma_sems.append(tgt)
        tgt.sem[1] += 16
        eng.h.dma_start(out=out, in_=in_, **kw).then_inc(tgt.sem[0], 16)
        ev = (tgt.sem[0], tgt.sem[1])
        for b in reads:
            b.r.append(ev)
        for b in writes:
            b.w = ev
            b.r = []
        return ev

    def V(self, fn, reads=(), writes=()):
        return self.op(self.dve, fn, reads, writes)

    def A(self, fn, reads=(), writes=()):
        return self.op(self.act, fn, reads, writes)

    def T(self, fn, reads=(), writes=()):
        return self.op(self.pe, fn, reads, writes)

    def psum(self):
        p = self.ps[self.psi % len(self.ps)]
        self.psi += 1
        return p

    def barrier(self):
        for e in self.engs:
            for o in self.engs:
                if o is not e and o.cnt > e.seen.get(id(o.sem), 0):
                    e.h.wait_ge(o.sem, o.cnt)
                    e.seen[id(o.sem)] = o.cnt
            for b in self.dma_sems:
                if b.sem[1] > e.seen.get(id(b.sem[0]), 0):
                    e.h.wait_ge(b.sem[0], b.sem[1])
                    e.seen[id(b.sem[0])] = b.sem[1]
        for b in self.dma_sems:
            self.free_sems.append(b.sem)
            b.sem = None
        self.dma_sems = []


class TT:
    def __init__(self, t):
        self.t = t
        self.b = Buf()

    def __getitem__(self, idx):
        return self.t[idx]


def bc_last(ap, n):
    return bass.AP(ap.tensor, ap.offset, [list(x) for x in ap.ap] + [[0, n]])


def bc_mid(ap, n):
    a = [list(x) for x in ap.ap]
    return bass.AP(ap.tensor, ap.offset, [a[0], [0, n]] + a[1:])


def build(NP=4, NE=32, SAMPLE=True, DBG=False, LIM=99):
    nc = bass.Bass("TRN2", target_bir_lowering=False)
    NT = NP * 2 + (2 if SAMPLE else 0)
    NPT = NP * 2

    def din(name, shape, dt=F32):
        return nc.dram_tensor(name, list(shape), dt, kind="ExternalInput").ap()

    def dout(name, shape):
        return nc.dram_tensor(name, list(shape), F32, kind="ExternalOutput").ap()

    xp_d = din("xp", [NP * 256, D])
    xs_seq_d = din("xs_seq", [1024, D])
    xs_own_d = din("xs_own", [256, D])
    cvT_d = din("cvT", [128, 16, 2])
    wmod_d = din("w_mod", [D, 6 * D])
    bmod_d = din("b_mod", [1, 6 * D])
    win_d = din("w_in", [D, IN_COLS])
    convw_d = din("convw", [128, 24, 5])
    convb_d = din("convb", [128, 24])
    dtb_d = din("dtb", [1, 64])
    alog_d = din("alog", [1, 64])
    ssdd_d = din("ssdd", [1, 32])
    normw_d = din("normw", [128, 16])
    wss_d = din("w_ssd_out", [D, D])
    wna_d = din("w_na_out", [D, D])
    wo_d = din("w_o", [D, D])
    lnv_d = din("lnv", [4, D])
    wr_d = din("wr", [128, 16, 32])
    br_d = din("br", [1, 32])
    wgu_d = din("wgu", [NE, 16, 128, 16 * 256])
    bgu_d = din("bgu", [128, NE, 32])
    wdn_d = din("wdn", [NE, 4, 128, 16 * 512])
    bdn_d = din("bdn", [NE, D])
    cst_d = din("cst", [128, 4, 128])
    if SAMPLE:
        bias_d = din("nbias", [16, 256, 1024])
        kctxT_d = din("kctxT", [16, 128, 512])
        vctx_d = din("vctx", [512, D])
        s0_d = din("s0", [2, 128, D])
        sel_d = din("sel", [128, 16])
    yp_o = dout("y_p", [NP * 256, D])
    nk_o = dout("nk", [NP * 256, D])
    nv_o = dout("nv", [NP * 256, D])
    hf_o = dout("hf", [NP, D, 128])
    hb_o = dout("hb", [NP, D, 128])
    if SAMPLE:
        ys_o = dout("y_s", [256, D])
    x1_d = nc.dram_tensor("x1_scr", [NT * 128, D], F32, kind="Internal").ap()
    mods_d = nc.dram_tensor("mods_scr", [2, 6 * D], F32, kind="Internal").ap()
    dbg = {}
    if DBG:
        dbg["x1"] = dout("dbg_x1", [NT * 128, D])
        if SAMPLE:
            for nm in ("yn", "ys", "gs", "gn"):
                dbg[nm] = dout("dbg_" + nm, [128, 16, 256])

    with contextlib.ExitStack() as ges:
        k = K(nc, ges)
        nc_ = nc

        uid = [0]

        def sb(es, name, shape, dt=F32):
            uid[0] += 1
            return TT(es.enter_context(nc_.sbuf_tensor("%s_%d" % (name, uid[0]), list(shape), dt)))

        k.ps = [TT(ges.enter_context(nc.psum_tensor("ps%d" % i, [128, 512], F32))) for i in range(8)]
        V, A, T = k.V, k.A, k.T
        v, a, pe = nc.vector, nc.scalar, nc.tensor

        cst = sb(ges, "cst", [128, 4, 128])
        k.dma(k.sp, cst[:, :, :], cst_d[:, :, :], writes=[cst.b])
        ident, trif, trib, ones = cst[:, 0, :], cst[:, 1, :], cst[:, 2, :], cst[:, 3, :]
        eps_t = sb(ges, "eps", [128, 2])
        V(lambda: v.memset(eps_t[:, 0:1], LN_EPS), writes=[eps_t.b])
        V(lambda: v.memset(eps_t[:, 1:2], 1.0), writes=[eps_t.b])
        modT = sb(ges, "modT", [128, 96, 2])
        comb = sb(ges, "comb", [128, NT, 32])

        with contextlib.ExitStack() as es:
            cvT = sb(es, "cvT", [128, 16, 2])
            scT = sb(es, "scT", [128, 16, 2], BF16)
            sig = sb(es, "sigc", [128, 16, 2])
            mods = sb(es, "mods", [2, 6 * D])
            bm2 = sb(es, "bm2", [2, 6 * D])
            wm = [sb(es, "wm%d" % i, [128, 16, 512], BF16) for i in range(2)]
            k.dma(k.sp, cvT[:, :, :], cvT_d[:, :, :], writes=[cvT.b])
            k.dma(k.sp, bm2[:, :], bmod_d[0:1, :].to_broadcast([2, 6 * D]), writes=[bm2.b])
            A(lambda: a.activation(out=sig[:, :, :], in_=cvT[:, :, :], func=AF.Sigmoid), [cvT.b], [sig.b])
            V(lambda: v.tensor_tensor(out=scT[:, :, :], in0=cvT[:, :, :], in1=sig[:, :, :], op=ALU.mult), [cvT.b, sig.b], [scT.b])
            wmv = wmod_d.rearrange("(kc p) n -> p kc n", p=128)
            for ct in range(24):
                w = wm[ct % 2]
                k.dma(k.pool, w[:, :, :], wmv[:, :, ct * 512:(ct + 1) * 512], writes=[w.b], max_dma_last_dim=2048)
                ps = k.psum()

                def mm(ps=ps, w=w):
                    for kc in range(16):
                        r = pe.matmul(ps[0:2, :], scT[:, kc, :], w[:, kc, :], start=(kc == 0), stop=(kc == 15))
                    return r
                T(mm, [scT.b, w.b], [ps.b])
                V(lambda ps=ps, ct=ct: v.tensor_tensor(out=mods[0:2, ct * 512:(ct + 1) * 512], in0=ps[0:2, :],
                                                      in1=bm2[0:2, ct * 512:(ct + 1) * 512], op=ALU.add), [ps.b, bm2.b], [mods.b])
            k.dma(k.sp, mods_d[:, :], mods[0:2, :], reads=[mods.b])
            modsd_b = Buf()
            modsd_b.w = mods.b.r[-1]
            ps = k.psum()

            def tr(ps=ps):
                for c in range(96):
                    r = pe.transpose(ps[:, c * 2:(c + 1) * 2], mods[0:2, c * 128:(c + 1) * 128], cst[0:2, 0, 0:2])
                return r
            T(tr, [mods.b, cst.b], [ps.b])
            V(lambda: v.tensor_copy(out=modT[:, :, :], in_=ps[:, 0:192].rearrange("p (c t) -> p c t", t=2)), [ps.b], [modT.b])
            V(lambda: v.tensor_scalar_add(out=modT[:, 16:32, :], in0=modT[:, 16:32, :], scalar1=1.0), [modT.b], [modT.b])
            V(lambda: v.tensor_scalar_add(out=modT[:, 64:80, :], in0=modT[:, 64:80, :], scalar1=1.0), [modT.b], [modT.b])
            k.barrier()

        def ln_stats(es_tmp, xt, xb, mv, rstd):
            st = es_tmp
            for i in range(4):
                V(lambda i=i: v.bn_stats(out=st[:, i * 6:(i + 1) * 6], in_=xt[:, i * 512:(i + 1) * 512]), [xb], [st.b])
            V(lambda: v.bn_aggr(out=mv[:, 0:2], in_=st[:, 0:24]), [st.b], [mv.b])
            A(lambda: a.activation(out=rstd[:, 0:1], in_=mv[:, 1:2], func=AF.Sqrt, bias=eps_t[:, 0:1], scale=1.0), [mv.b, eps_t.b], [rstd.b])
            V(lambda: v.reciprocal(out=rstd[:, 0:1], in_=rstd[:, 0:1]), [rstd.b], [rstd.b])

        def normalize_T(xt, xb, xn, st, mv, rstd, cv, sh_c, sc_c, outs):
            ln_stats(st, xt, xb, mv, rstd)
            V(lambda: v.tensor_scalar(out=xn[:, :], in0=xt, scalar1=mv[:, 0:1], scalar2=rstd[:, 0:1],
                                      op0=ALU.subtract, op1=ALU.mult), [xb, mv.b, rstd.b], [xn.b])
            for c4 in range(4):
                ps = k.psum()

                def tr(ps=ps, c4=c4):
                    for j in range(4):
                        c = c4 * 4 + j
                        r = pe.transpose(ps[:, j * 128:(j + 1) * 128], xn[:, c * 128:(c + 1) * 128], ident)
                    return r
                T(tr, [xn.b, cst.b], [ps.b])
                for j in range(4):
                    c = c4 * 4 + j
                    for (apf, ob) in outs:
                        if (c + (0 if len(outs) == 1 else 0)) % 2 == 0:
                            V(lambda ps=ps, j=j, c=c, apf=apf: v.tensor_scalar(
                                out=apf(c), in0=ps[:, j * 128:(j + 1) * 128], scalar1=modT[:, sc_c + c, cv:cv + 1],
                                scalar2=modT[:, sh_c + c, cv:cv + 1], op0=ALU.mult, op1=ALU.add), [ps.b, modT.b], [ob])
                        else:
                            A(lambda ps=ps, j=j, c=c, apf=apf: a.activation(
                                out=apf(c), in_=ps[:, j * 128:(j + 1) * 128], func=AF.Identity,
                                bias=modT[:, sh_c + c, cv:cv + 1], scale=modT[:, sc_c + c, cv:cv + 1]), [ps.b, modT.b], [ob])

        winv = win_d.rearrange("(kc p) n -> p kc n", p=128)

        def unit(es, u, sample):
            TS = 1024 if sample else 256
            NCH = TS // 128
            cv = 1 if sample else 0
            row0 = NPT * 128 if sample else u * 256
            xown_d = xs_own_d if sample else xp_d[u * 256:(u + 1) * 256, :]
            xseq_d = xs_seq_d if sample else xown_d
            xn = sb(es, "xn", [128, D])
            st = sb(es, "st", [128, 24])
            mv = sb(es, "mv", [128, 2])
            rstd = sb(es, "rstd", [128, 1])
            wt = [sb(es, "wt%d" % i, [128, 16, 256], BF16) for i in range(2)]
            wti = [0]
            ynT = sb(es, "ynT", [128, 16, 256], BF16)
            ysT = sb(es, "ysT", [128, 16, 256], BF16)
            gsT = sb(es, "gsT", [128, 16, 256], BF16)
            gnT = sb(es, "gnT", [128, 16, 256], BF16)
            hT = sb(es, "hT", [128, 16, TS], BF16)
            hTo = sb(es, "hTo", [128, 16, 256], BF16) if sample else hT

            def wtile(dram_view, c0, ncols):
                w = wt[wti[0] % 2]
                wti[0] += 1
                k.dma(k.pool, w[:, :, 0:ncols], dram_view[:, :, c0:c0 + ncols], writes=[w.b], max_dma_last_dim=2048)
                return w

            def proj_fm(w, wc, src, tok0, ntok, ps):
                def mm():
                    for kc in range(16):
                        r = pe.matmul(ps[:, 0:ntok], w[:, kc, wc * 128:(wc + 1) * 128], src[:, kc, tok0:tok0 + ntok],
                                      start=(kc == 0), stop=(kc == 15))
                    return r
                T(mm, [w.b, src.b], [ps.b])

            def proj_tm(w, ncols, src, tt, ps):
                def mm():
                    for kc in range(16):
                        r = pe.matmul(ps[:, 0:ncols], src[:, kc, tt * 128:(tt + 1) * 128], w[:, kc, 0:ncols],
                                      start=(kc == 0), stop=(kc == 15))
                    return r
                T(mm, [w.b, src.b], [ps.b])

            with contextlib.ExitStack() as e1:
                xq = sb(e1, "xq", [128, D])
                if sample:
                    for tt in range(NCH):
                        k.dma(k.sp, xq[:, :], xseq_d[tt * 128:(tt + 1) * 128, :], writes=[xq.b])
                        normalize_T(xq[:, :], xq.b, xn, st, mv, rstd, cv, 0, 16,
                                    [(lambda c, tt=tt: hT[:, c, tt * 128:(tt + 1) * 128], hT.b)])
                for tt in range(2):
                    k.dma(k.sp, xq[:, :], xown_d[tt * 128:(tt + 1) * 128, :], writes=[xq.b])
                    normalize_T(xq[:, :], xq.b, xn, st, mv, rstd, cv, 0, 16,
                                [(lambda c, tt=tt: hTo[:, c, tt * 128:(tt + 1) * 128], hTo.b)])
            k.barrier()

            if LIM <= 2:
                return
            for (c0, gt) in ((C_GS, gsT), (C_GN, gnT)):
                for ct in range(8):
                    w = wtile(winv, c0 + ct * 256, 256)
                    for wc in range(2):
                        ps = k.psum()
                        proj_fm(w, wc, hTo, 0, 256, ps)
                        A(lambda ps=ps, gt=gt, cc=ct * 2 + wc: a.activation(out=gt[:, cc, :], in_=ps[:, 0:256], func=AF.Sigmoid), [ps.b], [gt.b])

            if LIM <= 3:
                return
            with contextlib.ExitStack() as ea:
                qT = sb(ea, "qT", [128, 16, 256], BF16)
                kT = sb(ea, "kT", [128, 16, TS], BF16)
                vb = sb(ea, "vb", [128, NCH, D], BF16)
                stg = [sb(ea, "stg%d" % i, [128, 256]) for i in range(2)]
                stgi = [0]
                SC = 128.0 ** -0.5
                for ct in range(8):
                    w = wtile(winv, C_Q + ct * 256, 256)
                    for wc in range(2):
                        ps = k.psum()
                        proj_fm(w, wc, hTo, 0, 256, ps)
                        A(lambda ps=ps, h=ct * 2 + wc: a.activation(out=qT[:, h, :], in_=ps[:, 0:256], func=AF.Identity, scale=SC), [ps.b], [qT.b])
                if LIM <= 3.1:
                    return
                for ct in range(8):
                    w = wtile(winv, C_K + ct * 256, 256)
                    for wc in range(2):
                        for t0 in range(0, TS, 512):
                            n = min(512, TS - t0)
                            ps = k.psum()
                            proj_fm(w, wc, hT, t0, n, ps)
                            V(lambda ps=ps, h=ct * 2 + wc, t0=t0, n=n: v.tensor_copy(out=kT[:, h, t0:t0 + n], in_=ps[:, 0:n]), [ps.b], [kT.b])
                    if not sample:
                        for tt in range(2):
                            ps = k.psum()
                            proj_tm(w, 256, hT, tt, ps)
                            s = stg[stgi[0] % 2]
                            stgi[0] += 1
                            A(lambda ps=ps, s=s: a.activation(func=AF.Identity, out=s[:, :], in_=ps[:, 0:256]), [ps.b], [s.b])
                            if LIM != 3.3:
                                k.dma(k.sp, nk_o[u * 256 + tt * 128:u * 256 + (tt + 1) * 128, ct * 256:(ct + 1) * 256], s[:, :], reads=[s.b])
                if LIM <= 3.2:
                    return
                for ct in range(8):
                    w = wtile(winv, C_V + ct * 256, 256)
                    for tt in range(NCH):
                        ps = k.psum()
                        proj_tm(w, 256, hT, tt, ps)
                        A(lambda ps=ps, tt=tt, ct=ct: a.activation(func=AF.Identity, out=vb[:, tt, ct * 256:(ct + 1) * 256], in_=ps[:, 0:256]), [ps.b], [vb.b])
                        if not sample:
                            s = stg[stgi[0] % 2]
                            stgi[0] += 1
                            A(lambda ps=ps, s=s: a.activation(func=AF.Identity, out=s[:, :], in_=ps[:, 0:256]), [ps.b], [s.b])
                            if LIM != 3.3:
                                k.dma(k.sp, nv_o[u * 256 + tt * 128:u * 256 + (tt + 1) * 128, ct * 256:(ct + 1) * 256], s[:, :], reads=[s.b])
                if LIM <= 3.3:
                    return
                NKB = (TS + (512 if sample else 0)) // 128
                NKEY = NKB * 128
                S = sb(ea, "S", [128, NKEY])
                PT = sb(ea, "PT", [128, NKB, 128], BF16)
                mx = sb(ea, "mx", [128, 2])
                rs = sb(ea, "rs", [128, 2])
                if sample:
                    kc_t = sb(ea, "kctx", [128, 512], BF16)
                    vc_t = sb(ea, "vctx", [128, 4, D], BF16)
                    bia = sb(ea, "bia", [128, 1024])
                    for t4 in range(4):
                        k.dma(k.pool, vc_t[:, t4, :], vctx_d[t4 * 128:(t4 + 1) * 128, :], writes=[vc_t.b], max_dma_last_dim=2048)
                for h in range(16):
                    if sample:
                        k.dma(k.pool, kc_t[:, :], kctxT_d[h, :, :], writes=[kc_t.b], max_dma_last_dim=2048)
                    for qt in range(2):
                        if sample:
                            k.dma(k.sp, bia[:, :], bias_d[h, qt * 128:(qt + 1) * 128, :], writes=[bia.b])
                        for kb in range(0, NKEY, 512):
                            n = min(512, NKEY - kb)
                            ps = k.psum()
                            if kb < TS:
                                T(lambda ps=ps, kb=kb, n=n: pe.matmul(ps[:, 0:n], qT[:, h, qt * 128:(qt + 1) * 128], kT[:, h, kb:kb + n], start=True, stop=True),
                                  [qT.b, kT.b], [ps.b])
                                if sample:
                                    V(lambda ps=ps, kb=kb, n=n: v.tensor_tensor(out=S[:, kb:kb + n], in0=ps[:, 0:n], in1=bia[:, kb:kb + n], op=ALU.add), [ps.b, bia.b], [S.b])
                                else:
                                    V(lambda ps=ps, kb=kb, n=n: v.tensor_copy(out=S[:, kb:kb + n], in_=ps[:, 0:n]), [ps.b], [S.b])
                            else:
                                T(lambda ps=ps, n=n: pe.matmul(ps[:, 0:n], qT[:, h, qt * 128:(qt + 1) * 128], kc_t[:, 0:n], start=True, stop=True),
                                  [qT.b, kc_t.b], [ps.b])
                                V(lambda ps=ps, kb=kb, n=n: v.tensor_copy(out=S[:, kb:kb + n], in_=ps[:, 0:n]), [ps.b], [S.b])
                        if LIM <= 3.5:
                            continue
                        V(lambda: v.reduce_max(out=mx[:, 0:1], in_=S[:, :], axis=AX.X), [S.b], [mx.b])
                        V(lambda: v.tensor_scalar(out=mx[:, 1:2], in0=mx[:, 0:1], scalar1=-1.0, scalar2=None, op0=ALU.mult), [mx.b], [mx.b])
                        A(lambda: a.activation(out=S[:, :], in_=S[:, :], func=AF.Exp, bias=mx[:, 1:2], scale=1.0, accum_out=rs[:, 0:1]), [S.b, mx.b], [S.b, rs.b])
                        V(lambda: v.reciprocal(out=rs[:, 1:2], in_=rs[:, 0:1]), [rs.b], [rs.b])
                        V(lambda: v.tensor_scalar(out=S[:, :], in0=S[:, :], scalar1=rs[:, 1:2], scalar2=None, op0=ALU.mult), [S.b, rs.b], [S.b])
                        if LIM <= 3.7:
                            continue
                        for k4 in range(0, NKB, 4):
                            ps = k.psum()
                            nn = min(4, NKB - k4)

                            def tr(ps=ps, k4=k4, nn=nn):
                                for j in range(nn):
                                    r = pe.transpose(ps[:, j * 128:(j + 1) * 128], S[:, (k4 + j) * 128:(k4 + j + 1) * 128], ident)
                                return r
                            T(tr, [S.b, cst.b], [ps.b])
                            if (k4 // 4) % 2 == 0:
                                V(lambda ps=ps, k4=k4, nn=nn: v.tensor_copy(out=PT[:, k4:k4 + nn, :], in_=ps[:, 0:nn * 128].rearrange("p (j q) -> p j q", q=128)), [ps.b], [PT.b])
                            else:
                                A(lambda ps=ps, k4=k4, nn=nn: a.activation(func=AF.Identity, out=PT[:, k4:k4 + nn, :], in_=ps[:, 0:nn * 128].rearrange("p (j q) -> p j q", q=128)), [ps.b], [PT.b])
                        ps = k.psum()

                        def pv(ps=ps):
                            for kb in range(NKB):
                                if kb < NCH:
                                    lhs = vb[:, kb, h * 128:(h + 1) * 128]
                                else:
                                    lhs = vc_t[:, kb - NCH, h * 128:(h + 1) * 128]
                                r = pe.matmul(ps[:, 0:128], lhs, PT[:, kb, :], start=(kb == 0), stop=(kb == NKB - 1))
                            return r
                        T(pv, [vb.b, PT.b] + ([vc_t.b] if sample else []), [ps.b])
                        A(lambda ps=ps: a.activation(func=AF.Identity, out=ynT[:, h, qt * 128:(qt + 1) * 128], in_=ps[:, 0:128]), [ps.b], [ynT.b])
                k.barrier()

            if LIM <= 4:
                return
            with contextlib.ExitStack() as e2:
                xtm = sb(e2, "xtm", [128, NCH, D], BF16)
                Btm = sb(e2, "Btm", [128, NCH, 512], BF16)
                BT = sb(e2, "BT", [128, 4, TS], BF16)
                CT = sb(e2, "CT", [128, 4, TS], BF16)
                dtt = sb(e2, "dtt", [128, NCH, 64])
                dAt = sb(e2, "dAt", [128, NCH, 64])
                yown = sb(e2, "yown", [128, 2, D])
                cw = sb(e2, "cw", [128, 24, 5])
                cb = sb(e2, "cb", [128, 24])
                dtb = sb(e2, "dtb", [128, 64])
                Abc = sb(e2, "Abc", [128, 64])
                dbc = sb(e2, "dbc", [128, 32])
                nw = sb(e2, "nw", [128, 16])
                k.dma(k.sp, cw[:, :, :], convw_d[:, :, :], writes=[cw.b])
                k.dma(k.sp, cb[:, :], convb_d[:, :], writes=[cb.b])
                k.dma(k.sp, dtb[:, :], dtb_d[0:1, :].to_broadcast([128, 64]), writes=[dtb.b])
                k.dma(k.sp, Abc[:, :], alog_d[0:1, :].to_broadcast([128, 64]), writes=[Abc.b])
                k.dma(k.sp, dbc[:, :], ssdd_d[0:1, :].to_broadcast([128, 32]), writes=[dbc.b])
                k.dma(k.sp, nw[:, :], normw_d[:, :], writes=[nw.b])
                A(lambda: a.activation(out=Abc[:, :], in_=Abc[:, :], func=AF.Exp), [Abc.b], [Abc.b])
                V(lambda: v.tensor_scalar(out=Abc[:, :], in0=Abc[:, :], scalar1=-1.0, scalar2=None, op0=ALU.mult), [Abc.b], [Abc.b])
                if sample:
                    sel = sb(e2, "sel", [128, 16])
                    k.dma(k.sp, sel[:, :], sel_d[:, :], writes=[sel.b])
                with contextlib.ExitStack() as e3:
                    xpad = [sb(e3, "xpad%d" % i, [128, TS + 4]) for i in range(2)]
                    cacc = [sb(e3, "cacc%d" % i, [128, TS]) for i in range(2)]
                    for xp_ in xpad:
                        V(lambda xp_=xp_: v.memset(xp_[:, :], 0.0), writes=[xp_.b])
                    for ct in range(12):
                        w = wtile(winv, C_XBC + ct * 256, 256)
                        for wc in range(2):
                            c = ct * 2 + wc
                            xp_ = xpad[c % 2]
                            ca = cacc[c % 2]
                            for t0 in range(0, TS, 512):
                                n = min(512, TS - t0)
                                ps = k.psum()
                                proj_fm(w, wc, hT, t0, n, ps)
                                A(lambda ps=ps, xp_=xp_, t0=t0, n=n: a.activation(func=AF.Identity, out=xp_[:, 2 + t0:2 + t0 + n], in_=ps[:, 0:n]), [ps.b], [xp_.b])
                            V(lambda xp_=xp_, ca=ca, c=c: v.tensor_scalar(out=ca[:, :], in0=xp_[:, 0:TS], scalar1=cw[:, c, 0:1], scalar2=None, op0=ALU.mult),
                              [xp_.b, cw.b], [ca.b])
                            for j in range(1, 5):
                                V(lambda xp_=xp_, ca=ca, c=c, j=j: v.scalar_tensor_tensor(out=ca[:, :], in0=xp_[:, j:j + TS], scalar=cw[:, c, j:j + 1], in1=ca[:, :],
                                                                                         op0=ALU.mult, op1=ALU.add), [xp_.b, cw.b, ca.b], [ca.b])
                            if c < 20:
                                A(lambda ca=ca, c=c: a.activation(out=ca[:, :], in_=ca[:, :], func=AF.Silu, bias=cb[:, c:c + 1], scale=1.0), [ca.b, cb.b], [ca.b])
                                if c >= 16:
                                    V(lambda ca=ca, c=c: v.tensor_copy(out=BT[:, c - 16, :], in_=ca[:, :]), [ca.b], [BT.b])
                                for t4 in range(0, NCH, 4):
                                    nn = min(4, NCH - t4)
                                    ps = k.psum()

                                    def tr(ps=ps, t4=t4, nn=nn, ca=ca):
                                        for j in range(nn):
                                            r = pe.transpose(ps[:, j * 128:(j + 1) * 128], ca[:, (t4 + j) * 128:(t4 + j + 1) * 128], ident)
                                        return r
                                    T(tr, [ca.b, cst.b], [ps.b])
                                    dst = xtm if c < 16 else Btm
                                    col = c * 128 if c < 16 else (c - 16) * 128
                                    V(lambda ps=ps, t4=t4, nn=nn, dst=dst, col=col: v.tensor_copy(
                                        out=dst[:, t4:t4 + nn, col:col + 128], in_=ps[:, 0:nn * 128].rearrange("p (j q) -> p j q", q=128)), [ps.b], [dst.b])
                            else:
                                A(lambda ca=ca, c=c: a.activation(out=CT[:, c - 20, :], in_=ca[:, :], func=AF.Silu, bias=cb[:, c:c + 1], scale=1.0), [ca.b, cb.b], [CT.b])
                    w = wtile(winv, C_DT, 64)
                    t1 = sb(e3, "t1", [128, 64])
                    t2 = sb(e3, "t2", [128, 64])
                    for tt in range(NCH):
                        ps = k.psum()
                        proj_tm(w, 64, hT, tt, ps)
                        V(lambda ps=ps: v.tensor_tensor(out=t1[:, :], in0=ps[:, 0:64], in1=dtb[:, :], op=ALU.add), [ps.b, dtb.b], [t1.b])
                        A(lambda: a.activation(out=t2[:, :], in_=t1[:, :], func=AF.Abs), [t1.b], [t2.b])
                        A(lambda: a.activation(out=t2[:, :], in_=t2[:, :], func=AF.Exp, scale=-1.0), [t2.b], [t2.b])
                        A(lambda: a.activation(out=t2[:, :], in_=t2[:, :], func=AF.Ln, bias=eps_t[:, 1:2], scale=1.0), [t2.b, eps_t.b], [t2.b])
                        V(lambda tt=tt: v.scalar_tensor_tensor(out=dtt[:, tt, :], in0=t1[:, :], scalar=0.0, in1=t2[:, :], op0=ALU.max, op1=ALU.add), [t1.b, t2.b], [dtt.b])
                        V(lambda tt=tt: v.tensor_tensor(out=dAt[:, tt, :], in0=dtt[:, tt, :], in1=Abc[:, :], op=ALU.mult), [dtt.b, Abc.b], [dAt.b])
                    k.barrier()
                if LIM <= 5:
                    return
                if sample:
                    V(lambda: v.memset(yown[:, :, :], 0.0), writes=[yown.b])
                for d in range(2):
                    with contextlib.ExitStack() as e3:
                        STd = sb(e3, "ST", [128, D])
                        STbd = sb(e3, "STb", [128, D], BF16)
                        CBm = sb(e3, "CBm", [128, 4, 128])
                        nacs = sb(e3, "nacs", [128, 32])
                        eacs = sb(e3, "eacs", [128, 32])
                        etot = sb(e3, "etot", [128, 32])
                        coef = sb(e3, "coef", [128, 32])
                        xdt = sb(e3, "xdt", [128, D], BF16)
                        xdd = sb(e3, "xdd", [128, D], BF16)
                        Eh = [sb(e3, "Eh%d" % i, [128, 128]) for i in range(2)]
                        Gh = [sb(e3, "Gh%d" % i, [128, 128], BF16) for i in range(4)]
                        ytmp = sb(e3, "ytmp", [128, 512])
                        ytmp2 = sb(e3, "ytmp2", [128, 512])
                        if sample:
                            k.dma(k.sp, STd[:, :], s0_d[d, :, :], writes=[STd.b])
                        else:
                            V(lambda: v.memset(STd[:, :], 0.0), writes=[STd.b])
                        V(lambda: v.tensor_copy(out=STbd[:, :], in_=STd[:, :]), [STd.b], [STbd.b])
                        tri = trif if d == 0 else trib
                        for c in (range(NCH) if d == 0 else range(NCH - 1, -1, -1)):
                            tok = slice(c * 128, (c + 1) * 128)
                            psA = k.psum()
                            T(lambda psA=psA, c=c, tri=tri: pe.matmul(psA[:, 0:32], tri, dAt[:, c, d * 32:(d + 1) * 32], start=True, stop=True),
                              [cst.b, dAt.b], [psA.b])
                            psT = k.psum()
                            T(lambda psT=psT, c=c: pe.matmul(psT[:, 0:32], ones, dAt[:, c, d * 32:(d + 1) * 32], start=True, stop=True),
                              [cst.b, dAt.b], [psT.b])
                            V(lambda psA=psA: v.tensor_scalar(out=nacs[:, :], in0=psA[:, 0:32], scalar1=-1.0, scalar2=None, op0=ALU.mult), [psA.b], [nacs.b])
                            A(lambda psA=psA: a.activation(out=eacs[:, :], in_=psA[:, 0:32], func=AF.Exp), [psA.b], [eacs.b])
                            A(lambda psT=psT: a.activation(out=etot[:, :], in_=psT[:, 0:32], func=AF.Exp), [psT.b], [etot.b])
                            V(lambda psT=psT: v.tensor_tensor(out=coef[:, :], in0=psT[:, 0:32], in1=nacs[:, :], op=ALU.add), [psT.b, nacs.b], [coef.b])
                            A(lambda: a.activation(out=coef[:, :], in_=coef[:, :], func=AF.Exp), [coef.b], [coef.b])
                            V(lambda c=c: v.tensor_tensor(out=coef[:, :], in0=coef[:, :], in1=dtt[:, c, d * 32:(d + 1) * 32], op=ALU.mult), [coef.b, dtt.b], [coef.b])
                            xv = xtm[:, c, :].rearrange("p (h q) -> p h q", q=64)
                            V(lambda c=c, xv=xv: v.tensor_tensor(out=xdt[:, :].rearrange("p (h q) -> p h q", q=64), in0=xv,
                                                                 in1=bc_last(dtt[:, c, d * 32:(d + 1) * 32], 64), op=ALU.mult), [xtm.b, dtt.b], [xdt.b])
                            V(lambda xv=xv: v.tensor_tensor(out=xdd[:, :].rearrange("p (h q) -> p h q", q=64), in0=xv,
                                                            in1=bc_last(coef[:, :], 64), op=ALU.mult), [xtm.b, coef.b], [xdd.b])
                            psC = k.psum()

                            def cbm(psC=psC, tok=tok):
                                for g in range(4):
                                    r = pe.matmul(psC[:, g * 128:(g + 1) * 128], BT[:, g, tok], CT[:, g, tok], start=True, stop=True)
                                return r
                            T(cbm, [BT.b, CT.b], [psC.b])
                            V(lambda psC=psC, tri=tri: v.tensor_tensor(out=CBm[:, :, :], in0=psC[:, :].rearrange("p (g i) -> p g i", i=128),
                                                                     in1=bc_mid(tri, 4), op=ALU.mult), [psC.b, cst.b], [CBm.b])
                            for g in range(4):
                                psY, psO, psS = k.psum(), k.psum(), k.psum()
                                for h4 in range(2):
                                    psR = k.psum()

                                    def rr(psR=psR, h4=h4, g=g, c=c, tri=tri):
                                        for j in range(4):
                                            hh = d * 32 + g * 8 + h4 * 4 + j
                                            r = pe.matmul(psR[:, j * 128:(j + 1) * 128], dAt[:, c, hh:hh + 1].to_broadcast([128, 128]), tri, start=True, stop=True)
                                        return r
                                    T(rr, [dAt.b, cst.b], [psR.b])
                                    for j in range(4):
                                        hl = g * 8 + h4 * 4 + j
                                        E = Eh[j % 2]
                                        G = Gh[j]
                                        A(lambda psR=psR, j=j, hl=hl, E=E: a.activation(out=E[:, :], in_=psR[:, j * 128:(j + 1) * 128], func=AF.Exp,
                                                                                      bias=nacs[:, hl:hl + 1], scale=1.0), [psR.b, nacs.b], [E.b])
                                        V(lambda E=E, G=G, g=g: v.scalar_tensor_tensor(out=G[:, :], in0=E[:, :], scalar=1.0, in1=CBm[:, g, :], op0=ALU.min, op1=ALU.mult),
                                          [E.b, CBm.b], [G.b])
                                        o = (h4 * 4 + j) * 64
                                        T(lambda psY=psY, G=G, hl=hl, o=o: pe.matmul(psY[:, o:o + 64], G[:, :], xdt[:, hl * 64:(hl + 1) * 64], start=True, stop=True),
                                          [G.b, xdt.b], [psY.b])
                                        T(lambda psO=psO, g=g, hl=hl, o=o, tok=tok: pe.matmul(psO[:, o:o + 64], CT[:, g, tok], STbd[:, hl * 64:(hl + 1) * 64], start=True, stop=True),
                                          [CT.b, STbd.b], [psO.b])
                                        T(lambda psS=psS, g=g, hl=hl, o=o, c=c: pe.matmul(psS[:, o:o + 64], Btm[:, c, g * 128:(g + 1) * 128], xdd[:, hl * 64:(hl + 1) * 64], start=True, stop=True),
                                          [Btm.b, xdd.b], [psS.b])
                                gs = slice(g * 512, (g + 1) * 512)
                                V(lambda psO=psO, g=g: v.tensor_tensor(out=ytmp[:, :].rearrange("p (h q) -> p h q", q=64), in0=psO[:, :].rearrange("p (h q) -> p h q", q=64),
                                                                      in1=bc_last(eacs[:, g * 8:(g + 1) * 8], 64), op=ALU.mult), [psO.b, eacs.b], [ytmp.b])
                                if sample:
                                    V(lambda psY=psY: v.tensor_tensor(out=ytmp[:, :], in0=ytmp[:, :], in1=psY[:, :], op=ALU.add), [ytmp.b, psY.b], [ytmp.b])
                                    for sl in range(2):
                                        V(lambda sl=sl, c=c, gs=gs: v.scalar_tensor_tensor(out=yown[:, sl, gs], in0=ytmp[:, :], scalar=sel[:, c * 2 + sl:c * 2 + sl + 1],
                                                                                           in1=yown[:, sl, gs], op0=ALU.mult, op1=ALU.add), [ytmp.b, sel.b, yown.b], [yown.b])
                                else:
                                    if d == 0:
                                        V(lambda psY=psY, c=c, gs=gs: v.tensor_tensor(out=yown[:, c, gs], in0=ytmp[:, :], in1=psY[:, :], op=ALU.add), [ytmp.b, psY.b], [yown.b])
                                    else:
                                        V(lambda psY=psY: v.tensor_tensor(out=ytmp[:, :], in0=ytmp[:, :], in1=psY[:, :], op=ALU.add), [ytmp.b, psY.b], [ytmp.b])
                                        V(lambda c=c, gs=gs: v.tensor_tensor(out=yown[:, c, gs], in0=yown[:, c, gs], in1=ytmp[:, :], op=ALU.add), [ytmp.b, yown.b], [yown.b])
                                V(lambda g=g, gs=gs: v.tensor_tensor(out=ytmp2[:, :].rearrange("p (h q) -> p h q", q=64), in0=STd[:, gs].rearrange("p (h q) -> p h q", q=64),
                                                                     in1=bc_last(etot[:, g * 8:(g + 1) * 8], 64), op=ALU.mult), [STd.b, etot.b], [ytmp2.b])
                                V(lambda psS=psS, gs=gs: v.tensor_tensor(out=STd[:, gs], in0=ytmp2[:, :], in1=psS[:, :], op=ALU.add), [ytmp2.b, psS.b], [STd.b])
                            A(lambda: a.activation(func=AF.Identity, out=STbd[:, :], in_=STd[:, :]), [STd.b], [STbd.b])
                        if not sample:
                            so = sb(e3, "so", [128, 16, 128])
                            for c4 in range(4):
                                ps = k.psum()

                                def tr(ps=ps, c4=c4):
                                    for j in range(4):
                                        cc = c4 * 4 + j
                                        r = pe.transpose(ps[:, j * 128:(j + 1) * 128], STd[:, cc * 128:(cc + 1) * 128], ident)
                                    return r
                                T(tr, [STd.b, cst.b], [ps.b])
                                V(lambda ps=ps, c4=c4: v.tensor_copy(out=so[:, c4 * 4:(c4 + 1) * 4, :], in_=ps[:, :].rearrange("p (j q) -> p j q", q=128)), [ps.b], [so.b])
                            dst = (hf_o if d == 0 else hb_o)[u].rearrange("(c p) n -> p c n", p=128)
                            k.dma(k.sp, dst, so[:, :, :], reads=[so.b])
                        k.barrier()
                if LIM <= 6:
                    return
                with contextlib.ExitStack() as e3:
                    zs = sb(e3, "zs", [128, 2, D], BF16)
                    xso = sb(e3, "xso", [128, D])
                    ss = sb(e3, "ss", [128, 2])
                    for ct in range(8):
                        w = wtile(winv, C_Z + ct * 256, 256)
                        for tt in range(2):
                            ps = k.psum()
                            proj_tm(w, 256, hTo, tt, ps)
                            A(lambda ps=ps, tt=tt, ct=ct: a.activation(out=zs[:, tt, ct * 256:(ct + 1) * 256], in_=ps[:, 0:256], func=AF.Silu), [ps.b], [zs.b])
                    for tt in range(2):
                        if sample:
                            V(lambda: v.memset(xso[:, :], 0.0), writes=[xso.b])
                            for c in range(NCH):
                                V(lambda c=c, tt=tt: v.scalar_tensor_tensor(out=xso[:, :], in0=xtm[:, c, :], scalar=sel[:, c * 2 + tt:c * 2 + tt + 1], in1=xso[:, :],
                                                                            op0=ALU.mult, op1=ALU.add), [xtm.b, sel.b, xso.b], [xso.b])
                            xsrc = xso[:, :]
                            xsb = xso.b
                        else:
                            xsrc = xtm[:, tt, :]
                            xsb = xtm.b
                        V(lambda xsrc=xsrc: v.tensor_tensor(out=xn[:, :].rearrange("p (h q) -> p h q", q=64), in0=xsrc.rearrange("p (h q) -> p h q", q=64),
                                                            in1=bc_last(dbc[:, :], 64), op=ALU.mult), [xsb, dbc.b], [xn.b])
                        V(lambda tt=tt: v.tensor_tensor(out=xn[:, :], in0=xn[:, :], in1=yown[:, tt, :], op=ALU.add), [xn.b, yown.b], [xn.b])
                        V(lambda tt=tt: v.tensor_tensor(out=xn[:, :], in0=xn[:, :], in1=zs[:, tt, :], op=ALU.mult), [xn.b, zs.b], [xn.b])
                        A(lambda tt=tt: a.activation(out=yown[:, tt, :], in_=xn[:, :], func=AF.Square, accum_out=ss[:, 0:1]), [xn.b], [yown.b, ss.b])
                        A(lambda: a.activation(out=ss[:, 1:2], in_=ss[:, 0:1], func=AF.Sqrt, bias=eps_t[:, 0:1], scale=1.0 / D), [ss.b, eps_t.b], [ss.b])
                        V(lambda: v.reciprocal(out=ss[:, 1:2], in_=ss[:, 1:2]), [ss.b], [ss.b])
                        V(lambda: v.tensor_scalar(out=xn[:, :], in0=xn[:, :], scalar1=ss[:, 1:2], scalar2=None, op0=ALU.mult), [xn.b, ss.b], [xn.b])
                        for c4 in range(4):
                            ps = k.psum()

                            def tr(ps=ps, c4=c4):
                                for j in range(4):
                                    cc = c4 * 4 + j
                                    r = pe.transpose(ps[:, j * 128:(j + 1) * 128], xn[:, cc * 128:(cc + 1) * 128], ident)
                                return r
                            T(tr, [xn.b, cst.b], [ps.b])
                            for j in range(4):
                                cc = c4 * 4 + j
                                V(lambda ps=ps, j=j, cc=cc, tt=tt: v.tensor_scalar(out=ysT[:, cc, tt * 128:(tt + 1) * 128], in0=ps[:, j * 128:(j + 1) * 128],
                                                                                   scalar1=nw[:, cc:cc + 1], scalar2=None, op0=ALU.mult), [ps.b, nw.b], [ysT.b])
                k.barrier()
            if LIM <= 7:
                return
            if DBG and sample:
                for nm, tns in (("yn", ynT), ("ys", ysT), ("gs", gsT), ("gn", gnT)):
                    k.dma(k.pool, dbg[nm][:, :, :], tns[:, :, :], reads=[tns.b])
            with contextlib.ExitStack() as e3:
                xo = sb(e3, "xo", [128, 2, D])
                m2bc = sb(e3, "m2bc", [128, D])
                lnbc = sb(e3, "lnbc", [128, 2, D])
                mixT = sb(e3, "mixT", [128, 16, 256], BF16)
                m1 = sb(e3, "m1", [128, 256])
                m2 = sb(e3, "m2", [128, 256])
                uu = sb(e3, "uu", [128, 2, D])
                tmp = sb(e3, "tmpo", [128, 256])
                for tt in range(2):
                    k.dma(k.sp, xo[:, tt, :], xown_d[tt * 128:(tt + 1) * 128, :], writes=[xo.b])
                k.dma(k.sp, m2bc[:, :], mods_d[cv:cv + 1, 2 * D:3 * D].to_broadcast([128, D]), reads=[modsd_b], writes=[m2bc.b])
                k.dma(k.sp, lnbc[:, 0, :], lnv_d[0:1, :].to_broadcast([128, D]), writes=[lnbc.b])
                k.dma(k.sp, lnbc[:, 1, :], lnv_d[1:2, :].to_broadcast([128, D]), writes=[lnbc.b])
                wnav = wna_d.rearrange("(kc p) n -> p kc n", p=128)
                wssv = wss_d.rearrange("(kc p) n -> p kc n", p=128)
                wov = wo_d.rearrange("(kc p) n -> p kc n", p=128)
                for ct in range(8):
                    wa = wtile(wnav, ct * 256, 256)
                    wb_ = wtile(wssv, ct * 256, 256)
                    for wc in range(2):
                        cc = ct * 2 + wc
                        ps1, ps2 = k.psum(), k.psum()
                        proj_fm(wa, wc, ynT, 0, 256, ps1)
                        proj_fm(wb_, wc, ysT, 0, 256, ps2)
                        V(lambda ps1=ps1, cc=cc: v.tensor_tensor(out=m1[:, :], in0=ps1[:, 0:256], in1=gnT[:, cc, :], op=ALU.mult), [ps1.b, gnT.b], [m1.b])
                        V(lambda ps2=ps2, cc=cc: v.tensor_tensor(out=m2[:, :], in0=ps2[:, 0:256], in1=gsT[:, cc, :], op=ALU.mult), [ps2.b, gsT.b], [m2.b])
                        V(lambda cc=cc: v.tensor_tensor(out=mixT[:, cc, :], in0=m1[:, :], in1=m2[:, :], op=ALU.add), [m1.b, m2.b], [mixT.b])
                for ct in range(8):
                    w = wtile(wov, ct * 256, 256)
                    cs = slice(ct * 256, (ct + 1) * 256)
                    for tt in range(2):
                        ps = k.psum()
                        proj_tm(w, 256, mixT, tt, ps)
                        V(lambda ps=ps, cs=cs: v.tensor_tensor(out=tmp[:, :], in0=ps[:, 0:256], in1=m2bc[:, cs], op=ALU.mult), [ps.b, m2bc.b], [tmp.b])
                        V(lambda tt=tt, cs=cs: v.scalar_tensor_tensor(out=uu[:, tt, cs], in0=xo[:, tt, cs], scalar=DN_ALPHA, in1=tmp[:, :], op0=ALU.mult, op1=ALU.add),
                          [xo.b, tmp.b], [uu.b])
                for tt in range(2):
                    ln_stats(st, uu[:, tt, :], uu.b, mv, rstd)
                    V(lambda tt=tt: v.tensor_scalar(out=xn[:, :], in0=uu[:, tt, :], scalar1=mv[:, 0:1], scalar2=rstd[:, 0:1], op0=ALU.subtract, op1=ALU.mult),
                      [uu.b, mv.b, rstd.b], [xn.b])
                    V(lambda: v.tensor_tensor(out=xn[:, :], in0=xn[:, :], in1=lnbc[:, 0, :], op=ALU.mult), [xn.b, lnbc.b], [xn.b])
                    V(lambda: v.tensor_tensor(out=xn[:, :], in0=xn[:, :], in1=lnbc[:, 1, :], op=ALU.add), [xn.b, lnbc.b], [xn.b])
                    k.dma(k.sp, x1_d[row0 + tt * 128:row0 + (tt + 1) * 128, :], xn[:, :], reads=[xn.b])
                    if DBG:
                        k.dma(k.sp, dbg["x1"][row0 + tt * 128:row0 + (tt + 1) * 128, :], xn[:, :], reads=[xn.b])

        for u in range(NP if LIM > 1 else 0):
            with contextlib.ExitStack() as es:
                unit(es, u, False)
                k.barrier()
        if SAMPLE and LIM > 1:
            with contextlib.ExitStack() as es:
                unit(es, 0, True)
                k.barrier()

        halves = [list(range(0, (NT + 1) // 2)), list(range((NT + 1) // 2, NT))]
        for half in halves:
            if not half or LIM <= 8:
                continue
            nt = len(half)
            NTOK = nt * 128
            with contextlib.ExitStack() as es:
                xT = sb(es, "xT", [128, 16, NTOK], BF16)
                hid = sb(es, "hid", [128, 16, NTOK], BF16)
                acc = sb(es, "acc", [128, nt, D])
                wg = [sb(es, "wg%d" % i, [128, 16, 256], BF16) for i in range(2)]
                wd = [sb(es, "wd%d" % i, [128, 16, 512], BF16) for i in range(2)]
                bgu = sb(es, "bgu", [128, NE, 32])
                k.dma(k.sp, bgu[:, :, :], bgu_d[:, :, :], writes=[bgu.b])
                V(lambda: v.memset(acc[:, :, :], 0.0), writes=[acc.b])
                with contextlib.ExitStack() as e2:
                    x1t = sb(e2, "x1t", [128, D])
                    xn = sb(e2, "xnB", [128, D])
                    x2T = sb(e2, "x2T", [128, 16, 128])
                    st = sb(e2, "stB", [128, 24])
                    mv = sb(e2, "mvB", [128, 2])
                    rstd = sb(e2, "rstdB", [128, 1])
                    wr = sb(e2, "wr", [128, 16, 32])
                    brb = sb(e2, "brb", [128, 32])
                    lg = sb(e2, "lg", [128, 32])
                    t8 = sb(e2, "t8", [128, 8])
                    msk = sb(e2, "msk", [128, 32])
                    sm = sb(e2, "sm", [128, 2])
                    k.dma(k.sp, wr[:, :, :], wr_d[:, :, :], writes=[wr.b])
                    k.dma(k.sp, brb[:, :], br_d[0:1, :].to_broadcast([128, 32]), writes=[brb.b])
                    x1b = Buf()
                    for i, tg in enumerate(half):
                        cv = 0 if tg < NPT else 1
                        k.dma(k.sp, x1t[:, :], x1_d[tg * 128:(tg + 1) * 128, :], writes=[x1t.b])
                        normalize_T(x1t[:, :], x1t.b, xn, st, mv, rstd, cv, 48, 64,
                                    [(lambda c: x2T[:, c, :], x2T.b)])
                        V(lambda i=i: v.tensor_copy(out=xT[:, :, i * 128:(i + 1) * 128], in_=x2T[:, :, :]), [x2T.b], [xT.b])
                        ps = k.psum()

                        def rt(ps=ps):
                            for kc in range(16):
                                r = pe.matmul(ps[:, 0:32], x2T[:, kc, :], wr[:, kc, :], start=(kc == 0), stop=(kc == 15))
                            return r
                        T(rt, [x2T.b, wr.b], [ps.b])
                        V(lambda ps=ps: v.tensor_tensor(out=lg[:, :], in0=ps[:, 0:32], in1=brb[:, :], op=ALU.add), [ps.b, brb.b], [lg.b])
                        V(lambda: v.max(out=t8[:, :], in_=lg[:, :]), [lg.b], [t8.b])
                        V(lambda: v.tensor_scalar(out=msk[:, :], in0=lg[:, :], scalar1=t8[:, 3:4], scalar2=None, op0=ALU.is_ge), [lg.b, t8.b], [msk.b])
                        V(lambda: v.tensor_scalar(out=sm[:, 0:1], in0=t8[:, 0:1], scalar1=-1.0, scalar2=None, op0=ALU.mult), [t8.b], [sm.b])
                        A(lambda: a.activation(out=lg[:, :], in_=lg[:, :], func=AF.Exp, bias=sm[:, 0:1], scale=1.0), [lg.b, sm.b], [lg.b])
                        V(lambda: v.tensor_tensor(out=lg[:, :], in0=lg[:, :], in1=msk[:, :], op=ALU.mult), [lg.b, msk.b], [lg.b])
                        V(lambda: v.reduce_sum(out=sm[:, 1:2], in_=lg[:, :], axis=AX.X), [lg.b], [sm.b])
                        V(lambda: v.reciprocal(out=sm[:, 1:2], in_=sm[:, 1:2]), [sm.b], [sm.b])
                        V(lambda tg=tg: v.tensor_scalar(out=comb[:, tg, :], in0=lg[:, :], scalar1=sm[:, 1:2], scalar2=None, op0=ALU.mult), [lg.b, sm.b], [comb.b])
                    k.barrier()
                with contextlib.ExitStack() as e2:
                    g1 = [sb(e2, "g1_%d" % i, [128, 512]) for i in range(2)]
                    sg = [sb(e2, "sg_%d" % i, [128, 512]) for i in range(2)]
                    u0 = [sb(e2, "u0_%d" % i, [128, 512]) for i in range(2)]
                    gi = [0]
                    tgs = [(t0, min(512, NTOK - t0)) for t0 in range(0, NTOK, 512)]
                    for e in range(NE):
                        for j in range(16):
                            w = wg[j % 2]
                            k.dma(k.pool, w[:, :, :], wgu_d[e, j].rearrange("p (kc c) -> p kc c", c=256), writes=[w.b], max_dma_last_dim=2048)
                            for (t0, n) in tgs:
                                psg, psu = k.psum(), k.psum()

                                def mm(ps, off, w=w, t0=t0, n=n):
                                    for kc in range(16):
                                        r = pe.matmul(ps[:, 0:n], w[:, kc, off:off + 128], xT[:, kc, t0:t0 + n], start=(kc == 0), stop=(kc == 15))
                                    return r
                                T(lambda psg=psg, mm=mm: mm(psg, 0), [w.b, xT.b], [psg.b])
                                T(lambda psu=psu, mm=mm: mm(psu, 128), [w.b, xT.b], [psu.b])
                                i2 = gi[0] % 2
                                gi[0] += 1
                                G1, SG, U0 = g1[i2], sg[i2], u0[i2]
                                V(lambda psg=psg, G1=G1, n=n, e=e, j=j: v.tensor_scalar(out=G1[:, 0:n], in0=psg[:, 0:n], scalar1=bgu[:, e, j:j + 1], scalar2=7.0,
                                                                                       op0=ALU.add, op1=ALU.min), [psg.b, bgu.b], [G1.b])
                                A(lambda G1=G1, SG=SG, n=n: a.activation(out=SG[:, 0:n], in_=G1[:, 0:n], func=AF.Sigmoid, scale=1.702), [G1.b], [SG.b])
                                A(lambda psu=psu, U0=U0, n=n, e=e, j=j: a.activation(out=U0[:, 0:n], in_=psu[:, 0:n], func=AF.Identity, bias=bgu[:, e, 16 + j:17 + j], scale=1.0),
                                  [psu.b, bgu.b], [U0.b])
                                V(lambda U0=U0, n=n: v.tensor_scalar(out=U0[:, 0:n], in0=U0[:, 0:n], scalar1=-7.0, scalar2=7.0, op0=ALU.max, op1=ALU.min), [U0.b], [U0.b])
                                V(lambda G1=G1, SG=SG, n=n: v.tensor_tensor(out=G1[:, 0:n], in0=G1[:, 0:n], in1=SG[:, 0:n], op=ALU.mult), [G1.b, SG.b], [G1.b])
                                V(lambda G1=G1, U0=U0, n=n, j=j, t0=t0: v.scalar_tensor_tensor(out=hid[:, j, t0:t0 + n], in0=U0[:, 0:n], scalar=1.0, in1=G1[:, 0:n],
                                                                                              op0=ALU.add, op1=ALU.mult), [U0.b, G1.b], [hid.b])
                        for ct in range(4):
                            w = wd[ct % 2]
                            k.dma(k.pool, w[:, :, :], wdn_d[e, ct].rearrange("p (j c) -> p j c", c=512), writes=[w.b], max_dma_last_dim=2048)
                            for i, tg in enumerate(half):
                                ps = k.psum()

                                def dn(ps=ps, w=w, i=i):
                                    for j in range(16):
                                        r = pe.matmul(ps[:, :], hid[:, j, i * 128:(i + 1) * 128], w[:, j, :], start=(j == 0), stop=(j == 15))
                                    return r
                                T(dn, [hid.b, w.b], [ps.b])
                                V(lambda ps=ps, i=i, tg=tg, ct=ct, e=e: v.scalar_tensor_tensor(out=acc[:, i, ct * 512:(ct + 1) * 512], in0=ps[:, :], scalar=comb[:, tg, e:e + 1],
                                                                                              in1=acc[:, i, ct * 512:(ct + 1) * 512], op0=ALU.mult, op1=ALU.add),
                                  [ps.b, comb.b, acc.b], [acc.b])
                    k.barrier()
                with contextlib.ExitStack() as e2:
                    bdn = sb(e2, "bdn", [32, D])
                    cT = sb(e2, "cT", [32, 128])
                    x1t = sb(e2, "x1f", [128, D])
                    m5bc = sb(e2, "m5bc", [128, 2, D])
                    lnbc = sb(e2, "ln2bc", [128, 2, D])
                    st = sb(e2, "stF", [128, 24])
                    mv = sb(e2, "mvF", [128, 2])
                    rstd = sb(e2, "rstdF", [128, 1])
                    if NE < 32:
                        V(lambda: v.memset(bdn[:, :], 0.0), writes=[bdn.b])
                    k.dma(k.sp, bdn[0:NE, :], bdn_d[:, :], writes=[bdn.b])
                    for cv in range(2):
                        k.dma(k.sp, m5bc[:, cv, :], mods_d[cv:cv + 1, 5 * D:6 * D].to_broadcast([128, D]), reads=[modsd_b], writes=[m5bc.b])
                        k.dma(k.sp, lnbc[:, cv, :], lnv_d[2 + cv:3 + cv, :].to_broadcast([128, D]), writes=[lnbc.b])
                    for i, tg in enumerate(half):
                        cv = 0 if tg < NPT else 1
                        ps = k.psum()
                        T(lambda ps=ps, tg=tg: pe.transpose(ps[0:32, 0:128], comb[:, tg, :], ident), [comb.b, cst.b], [ps.b])
                        V(lambda ps=ps: v.tensor_copy(out=cT[:, :], in_=ps[0:32, 0:128]), [ps.b], [cT.b])
                        k.dma(k.sp, x1t[:, :], x1_d[tg * 128:(tg + 1) * 128, :], writes=[x1t.b])
                        for ct in range(4):
                            cs = slice(ct * 512, (ct + 1) * 512)
                            ps = k.psum()
                            T(lambda ps=ps, cs=cs: pe.matmul(ps[:, :], cT[:, :], bdn[:, cs], start=True, stop=True), [cT.b, bdn.b], [ps.b])
                            V(lambda ps=ps, i=i, cs=cs: v.tensor_tensor(out=acc[:, i, cs], in0=acc[:, i, cs], in1=ps[:, :], op=ALU.add), [ps.b, acc.b], [acc.b])
                        V(lambda i=i, cv=cv: v.tensor_tensor(out=acc[:, i, :], in0=acc[:, i, :], in1=m5bc[:, cv, :], op=ALU.mult), [acc.b, m5bc.b], [acc.b])
                        V(lambda i=i: v.scalar_tensor_tensor(out=acc[:, i, :], in0=x1t[:, :], scalar=DN_ALPHA, in1=acc[:, i, :], op0=ALU.mult, op1=ALU.add), [x1t.b, acc.b], [acc.b])
                        ln_stats(st, acc[:, i, :], acc.b, mv, rstd)
                        V(lambda i=i: v.tensor_scalar(out=x1t[:, :], in0=acc[:, i, :], scalar1=mv[:, 0:1], scalar2=rstd[:, 0:1], op0=ALU.subtract, op1=ALU.mult),
                          [acc.b, mv.b, rstd.b], [x1t.b])
                        V(lambda: v.tensor_tensor(out=x1t[:, :], in0=x1t[:, :], in1=lnbc[:, 0, :], op=ALU.mult), [x1t.b, lnbc.b], [x1t.b])
                        V(lambda: v.tensor_tensor(out=x1t[:, :], in0=x1t[:, :], in1=lnbc[:, 1, :], op=ALU.add), [x1t.b, lnbc.b], [x1t.b])
                        if tg < NPT:
                            dst = yp_o[tg * 128:(tg + 1) * 128, :]
                        else:
                            dst = ys_o[(tg - NPT) * 128:(tg - NPT + 1) * 128, :]
                        k.dma(k.sp, dst, x1t[:, :], reads=[x1t.b])
                k.barrier()
        k.barrier()
    return nc


def _consts():
    c = np.zeros((128, 4, 128), np.float32)
    i = np.arange(128)
    c[:, 0, :] = np.eye(128, dtype=np.float32)
    c[:, 1, :] = (i[:, None] <= i[None, :]).astype(np.float32)
    c[:, 2, :] = (i[:, None] >= i[None, :]).astype(np.float32)
    c[:, 3, :] = 1.0
    return c


def _nbias(rpb, q):
    W, R, KR, KC = 64, 16, 8, 16
    col = np.arange(W)
    c0 = np.clip(col - KC // 2, 0, W - KC)
    colmask = (col[None, :] >= c0[:, None]) & (col[None, :] < c0[:, None] + KC)
    dc = np.clip(col[None, :] - col[:, None] + KC - 1, 0, 2 * KC - 2)
    out = np.full((16, 4, W, R, W), NEG, np.float32)
    for ri in range(4):
        r = 4 * q + ri
        r0 = int(np.clip(r - KR // 2, 0, R - KR))
        for kr in range(r0, r0 + KR):
            dr = kr - r + KR - 1
            blk = rpb[:, dr, :][:, dc]
            out[:, ri, :, kr, :] = np.where(colmask[None], blk, np.float32(NEG))
    return np.ascontiguousarray(out.reshape(16, 256, 1024))


def make_inputs(inp, core, NP=4, NE=32, SAMPLE=True, prompt_ids=None, sb=None, sq=None):
    f = lambda x: np.ascontiguousarray(x, dtype=np.float32)
    if prompt_ids is None:
        prompt_ids = list(range(core * NP, (core + 1) * NP))
    if sb is None:
        sb, sq = core // 4, core % 4
    m = {}
    m["xp"] = f(inp["x_prompt"][prompt_ids].reshape(NP * 256, D))
    m["xs_seq"] = f(inp["x_sample"][sb])
    m["xs_own"] = f(inp["x_sample"][sb, sq * 256:(sq + 1) * 256])
    cv = np.stack([inp["c_ctx"], inp["c"][sb]], axis=-1)
    m["cvT"] = f(cv.reshape(16, 128, 2).transpose(1, 0, 2))
    m["w_mod"] = f(inp["w_mod"][0])
    m["b_mod"] = f(inp["b_mod"][0][None])
    m["w_in"] = f(inp["w_in"][0])
    m["convw"] = f(inp["ssd_conv_w"][0].T.reshape(24, 128, 5).transpose(1, 0, 2))
    m["convb"] = f(inp["ssd_conv_b"][0].reshape(24, 128).T)
    m["dtb"] = f(inp["ssd_dt_bias"][0].reshape(1, 64))
    m["alog"] = f(inp["ssd_a_log"][0].reshape(1, 64))
    m["ssdd"] = f(inp["ssd_d"][0].reshape(1, 32))
    m["normw"] = f(inp["ssd_norm_w"][0].reshape(16, 128).T)
    m["w_ssd_out"] = f(inp["w_ssd_out"][0])
    m["w_na_out"] = f(inp["w_na_out"][0])
    m["w_o"] = f(inp["w_o"][0])
    m["lnv"] = f(np.stack([inp["ln1_g"][0], inp["ln1_b"][0], inp["ln2_g"][0], inp["ln2_b"][0]]))
    m["wr"] = f(inp["w_router"][0][:, :NE].reshape(16, 128, NE).transpose(1, 0, 2)) if NE == 32 else None
    if NE != 32:
        wr = np.zeros((128, 16, 32), np.float32)
        wr[:, :, :NE] = inp["w_router"][0][:, :NE].reshape(16, 128, NE).transpose(1, 0, 2)
        m["wr"] = wr
        br = np.full((1, 32), -1e4, np.float32)
        br[0, :NE] = inp["b_router"][0][:NE]
        m["br"] = br
    else:
        m["br"] = f(inp["b_router"][0][None])
    m["wgu"] = inp["_wgu"]
    m["bgu"] = inp["_bgu"]
    m["wdn"] = inp["_wdn"]
    m["bdn"] = f(inp["b_down"][0][:NE])
    m["cst"] = _consts()
    if SAMPLE:
        m["nbias"] = _nbias(np.asarray(inp["na_rpb"][0], np.float32), sq)
        m["kctxT"] = f(inp["cache_na_k"][sb, 0].transpose(1, 2, 0))
        m["vctx"] = f(inp["cache_na_v"][sb, 0].reshape(512, D))
        s0 = np.stack([inp["state_ssd_fwd"][sb, 0], inp["state_ssd_bwd"][sb, 0]])
        m["s0"] = f(s0.reshape(2, D, 128).transpose(0, 2, 1))
        sel = np.zeros((128, 16), np.float32)
        for sl in range(2):
            sel[:, (2 * sq + sl) * 2 + sl] = 1.0
        m["sel"] = sel
    return m


def prep_shared(inp, NE=32):
    wgu = np.asarray(inp["w_gate_up"][0][:NE], np.float32)
    w6 = wgu.reshape(NE, 16, 128, 2, 16, 128)
    inp["_wgu"] = np.ascontiguousarray(w6.transpose(0, 4, 2, 1, 3, 5)).reshape(NE, 16, 128, 16 * 256)
    bgu = np.asarray(inp["b_gate_up"][0][:NE], np.float32).reshape(NE, 32, 128)
    inp["_bgu"] = np.ascontiguousarray(bgu.transpose(2, 0, 1))
    wdn = np.asarray(inp["w_down"][0][:NE], np.float32).reshape(NE, 16, 128, 4, 512)
    inp["_wdn"] = np.ascontiguousarray(wdn.transpose(0, 3, 2, 1, 4)).reshape(NE, 4, 128, 16 * 512)


_NC_CACHE = {}


def kernel(**inputs):
    inp = {k_: np.asarray(v_) for k_, v_ in inputs.items()}
    prep_shared(inp)
    if "nc" not in _NC_CACHE:
        _NC_CACHE["nc"] = build()
    nc = _NC_CACHE["nc"]
    in_maps = [make_inputs(inp, c) for c in range(NCORES)]
    res = run_bass_kernel_spmd(nc, in_maps, core_ids=list(range(NCORES)))
    r = res.results
    y_p = np.concatenate([r[c]["y_p"].reshape(4, 256, D) for c in range(NCORES)], axis=0)
    y_s = np.stack([np.concatenate([r[b * 4 + q]["y_s"] for q in range(4)], axis=0) for b in range(2)], axis=0)
    nk = np.concatenate([r[c]["nk"].reshape(4, 1, 256, 16, 128) for c in range(NCORES)], axis=0)
    nv = np.concatenate([r[c]["nv"].reshape(4, 1, 256, 16, 128) for c in range(NCORES)], axis=0)
    hf = np.concatenate([r[c]["hf"].reshape(4, 1, 32, 64, 128) for c in range(NCORES)], axis=0)
    hb = np.concatenate([r[c]["hb"].reshape(4, 1, 32, 64, 128) for c in range(NCORES)], axis=0)
    return (y_p.astype(np.float32), y_s.astype(np.float32), nk.astype(np.float32), nv.astype(np.float32),
            hf.astype(np.float32), hb.astype(np.float32))
```

```python
import contextlib
import numpy as np
import concourse.bass as bass
import concourse.mybir as mybir
from concourse.bass_utils import run_bass_kernel_spmd

F32 = mybir.dt.float32
BF16 = mybir.dt.bfloat16
AF = mybir.ActivationFunctionType
ALU = mybir.AluOpType
AX = mybir.AxisListType

D = 2048
NCORES = 8
LN_EPS = 1e-5
DN_ALPHA = 2.0 ** 0.25
IN_COLS = 15424
C_Q, C_K, C_V, C_Z, C_XBC, C_DT, C_GS, C_GN = 0, 2048, 4096, 6144, 8192, 11264, 11328, 13376
NEG = -1.0e30


class Buf:
    __slots__ = ("w", "r", "sem")

    def __init__(self):
        self.w = None
        self.r = []
        self.sem = None


class Eng:
    def __init__(self, h, sem, name):
        self.h, self.sem, self.name = h, sem, name
        self.cnt = 0
        self.seen = {}


class K:
    def __init__(self, nc, es):
        self.nc, self.es = nc, es
        self.sems = []
        mk = lambda n: es.enter_context(nc.semaphore(n))
        self.pe = Eng(nc.tensor, mk("s_pe"), "pe")
        self.dve = Eng(nc.vector, mk("s_dve"), "dve")
        self.act = Eng(nc.scalar, mk("s_act"), "act")
        self.pool = Eng(nc.gpsimd, mk("s_pool"), "pool")
        self.sp = Eng(nc.sync, mk("s_sp"), "sp")
        self.engs = [self.pe, self.dve, self.act, self.pool, self.sp]
        self.dma_sems = []
        self.free_sems = []
        self.nsem = 0
        self.ps = []
        self.psi = 0

    def _waits(self, eng, reads, writes):
        evs = []
        for b in reads:
            if b.w is not None:
                evs.append(b.w)
        for b in writes:
            if b.w is not None:
                evs.append(b.w)
            evs.extend(b.r)
        need = {}
        for sem, val in evs:
            k = id(sem)
            if eng.seen.get(k, 0) >= val:
                continue
            if k not in need or need[k][1] < val:
                need[k] = (sem, val)
        for k, (sem, val) in need.items():
            if sem is eng.sem and eng is self.pe:
                continue
            eng.h.wait_ge(sem, val)
            eng.seen[k] = val

    def op(self, eng, fn, reads=(), writes=()):
        self._waits(eng, reads, writes)
        ins = fn()
        eng.cnt += 1
        ins.then_inc(eng.sem, 1)
        ev = (eng.sem, eng.cnt)
        eng.seen[id(eng.sem)] = max(eng.seen.get(id(eng.sem), 0), 0)
        for b in reads:
            b.r.append(ev)
        for b in writes:
            b.w = ev
            b.r = []
        return ev

    def dma(self, eng, out, in_, reads=(), writes=(), **kw):
        self._waits(eng, reads, writes)
        tgt = writes[0] if writes else reads[0]
        if tgt.sem is None:
            if self.free_sems:
                tgt.sem = self.free_sems.pop()
            else:
                self.nsem += 1
                tgt.sem = [self.es.enter_context(self.nc.semaphore("d%d" % self.nsem)), 0]
            self.dma_sems.append(tgt)
        tgt.sem[1] += 16
        eng.h.dma_start(out=out, in_=in_, **kw).then_inc(tgt.sem[0], 16)
        ev = (tgt.sem[0], tgt.sem[1])
        for b in reads:
            b.r.append(ev)
        for b in writes:
            b.w = ev
            b.r = []
        return ev

    def V(self, fn, reads=(), writes=()):
        return self.op(self.dve, fn, reads, writes)

    def A(self, fn, reads=(), writes=()):
        return self.op(self.act, fn, reads, writes)

    def T(self, fn, reads=(), writes=()):
        return self.op(self.pe, fn, reads, writes)

    def psum(self):
        p = self.ps[self.psi % len(self.ps)]
        self.psi += 1
        return p

    def barrier(self):
        for e in self.engs:
            for o in self.engs:
                if o is not e and o.cnt > e.seen.get(id(o.sem), 0):
                    e.h.wait_ge(o.sem, o.cnt)
                    e.seen[id(o.sem)] = o.cnt
            for b in self.dma_sems:
                if b.sem[1] > e.seen.get(id(b.sem[0]), 0):
                    e.h.wait_ge(b.sem[0], b.sem[1])
                    e.seen[id(b.sem[0])] = b.sem[1]
        for b in self.dma_sems:
            self.free_sems.append(b.sem)
            b.sem = None
        self.dma_sems = []


class TT:
    def __init__(self, t):
        self.t = t
        self.b = Buf()

    def __getitem__(self, idx):
        return self.t[idx]


def bc_last(ap, n):
    return bass.AP(ap.tensor, ap.offset, [list(x) for x in ap.ap] + [[0, n]])


def bc_mid(ap, n):
    a = [list(x) for x in ap.ap]
    return bass.AP(ap.tensor, ap.offset, [a[0], [0, n]] + a[1:])


def build(NP=4, NE=32, SAMPLE=True, DBG=False, LIM=99):
    nc = bass.Bass("TRN2", target_bir_lowering=False)
    NT = NP * 2 + (2 if SAMPLE else 0)
    NPT = NP * 2

    def din(name, shape, dt=F32):
        return nc.dram_tensor(name, list(shape), dt, kind="ExternalInput").ap()

    def dout(name, shape):
        return nc.dram_tensor(name, list(shape), F32, kind="ExternalOutput").ap()

    xp_d = din("xp", [NP * 256, D])
    xs_seq_d = din("xs_seq", [1024, D])
    xs_own_d = din("xs_own", [256, D])
    cvT_d = din("cvT", [128, 16, 2])
    wmod_d = din("w_mod", [D, 6 * D])
    bmod_d = din("b_mod", [1, 6 * D])
    win_d = din("w_in", [D, IN_COLS])
    convw_d = din("convw", [128, 24, 5])
    convb_d = din("convb", [128, 24])
    dtb_d = din("dtb", [1, 64])
    alog_d = din("alog", [1, 64])
    ssdd_d = din("ssdd", [1, 32])
    normw_d = din("normw", [128, 16])
    wss_d = din("w_ssd_out", [D, D])
    wna_d = din("w_na_out", [D, D])
    wo_d = din("w_o", [D, D])
    lnv_d = din("lnv", [4, D])
    wr_d = din("wr", [128, 16, 32])
    br_d = din("br", [1, 32])
    wgu_d = din("wgu", [NE, 16, 128, 16 * 256])
    bgu_d = din("bgu", [128, NE, 32])
    wdn_d = din("wdn", [NE, 4, 128, 16 * 512])
    bdn_d = din("bdn", [NE, D])
    cst_d = din("cst", [128, 4, 128])
    if SAMPLE:
        bias_d = din("nbias", [16, 256, 1024])
        kctxT_d = din("kctxT", [16, 128, 512])
        vctx_d = din("vctx", [512, D])
        s0_d = din("s0", [2, 128, D])
        sel_d = din("sel", [128, 16])
    yp_o = dout("y_p", [NP * 256, D])
    nk_o = dout("nk", [NP * 256, D])
    nv_o = dout("nv", [NP * 256, D])
    hf_o = dout("hf", [NP, D, 128])
    hb_o = dout("hb", [NP, D, 128])
    if SAMPLE:
        ys_o = dout("y_s", [256, D])
    x1_d = nc.dram_tensor("x1_scr", [NT * 128, D], F32, kind="Internal").ap()
    mods_d = nc.dram_tensor("mods_scr", [2, 6 * D], F32, kind="Internal").ap()
    dbg = {}
    if DBG:
        dbg["x1"] = dout("dbg_x1", [NT * 128, D])
        if SAMPLE:
            for nm in ("yn", "ys", "gs", "gn"):
                dbg[nm] = dout("dbg_" + nm, [128, 16, 256])

    with contextlib.ExitStack() as ges:
        k = K(nc, ges)
        nc_ = nc

        uid = [0]

        def sb(es, name, shape, dt=F32):
            uid[0] += 1
            return TT(es.enter_context(nc_.sbuf_tensor("%s_%d" % (name, uid[0]), list(shape), dt)))

        k.ps = [TT(ges.enter_context(nc.psum_tensor("ps%d" % i, [128, 512], F32))) for i in range(8)]
        V, A, T = k.V, k.A, k.T
        v, a, pe = nc.vector, nc.scalar, nc.tensor

        cst = sb(ges, "cst", [128, 4, 128])
        k.dma(k.sp, cst[:, :, :], cst_d[:, :, :], writes=[cst.b])
        ident, trif, trib, ones = cst[:, 0, :], cst[:, 1, :], cst[:, 2, :], cst[:, 3, :]
        eps_t = sb(ges, "eps", [128, 2])
        V(lambda: v.memset(eps_t[:, 0:1], LN_EPS), writes=[eps_t.b])
        V(lambda: v.memset(eps_t[:, 1:2], 1.0), writes=[eps_t.b])
        modT = sb(ges, "modT", [128, 96, 2])
        comb = sb(ges, "comb", [128, NT, 32])

        with contextlib.ExitStack() as es:
            cvT = sb(es, "cvT", [128, 16, 2])
            scT = sb(es, "scT", [128, 16, 2], BF16)
            sig = sb(es, "sigc", [128, 16, 2])
            mods = sb(es, "mods", [2, 6 * D])
            bm2 = sb(es, "bm2", [2, 6 * D])
            wm = [sb(es, "wm%d" % i, [128, 16, 512], BF16) for i in range(2)]
            k.dma(k.sp, cvT[:, :, :], cvT_d[:, :, :], writes=[cvT.b])
            k.dma(k.sp, bm2[:, :], bmod_d[0:1, :].to_broadcast([2, 6 * D]), writes=[bm2.b])
            A(lambda: a.activation(out=sig[:, :, :], in_=cvT[:, :, :], func=AF.Sigmoid), [cvT.b], [sig.b])
            V(lambda: v.tensor_tensor(out=scT[:, :, :], in0=cvT[:, :, :], in1=sig[:, :, :], op=ALU.mult), [cvT.b, sig.b], [scT.b])
            wmv = wmod_d.rearrange("(kc p) n -> p kc n", p=128)
            for ct in range(24):
                w = wm[ct % 2]
                k.dma(k.pool, w[:, :, :], wmv[:, :, ct * 512:(ct + 1) * 512], writes=[w.b], max_dma_last_dim=2048)
                ps = k.psum()

                def mm(ps=ps, w=w):
                    for kc in range(16):
                        r = pe.matmul(ps[0:2, :], scT[:, kc, :], w[:, kc, :], start=(kc == 0), stop=(kc == 15))
                    return r
                T(mm, [scT.b, w.b], [ps.b])
                V(lambda ps=ps, ct=ct: v.tensor_tensor(out=mods[0:2, ct * 512:(ct + 1) * 512], in0=ps[0:2, :],
                                                      in1=bm2[0:2, ct * 512:(ct + 1) * 512], op=ALU.add), [ps.b, bm2.b], [mods.b])
            k.dma(k.sp, mods_d[:, :], mods[0:2, :], reads=[mods.b])
            modsd_b = Buf()
            modsd_b.w = mods.b.r[-1]
            ps = k.psum()

            def tr(ps=ps):
                for c in range(96):
                    r = pe.transpose(ps[:, c * 2:(c + 1) * 2], mods[0:2, c * 128:(c + 1) * 128], cst[0:2, 0, 0:2])
                return r
            T(tr, [mods.b, cst.b], [ps.b])
            V(lambda: v.tensor_copy(out=modT[:, :, :], in_=ps[:, 0:192].rearrange("p (c t) -> p c t", t=2)), [ps.b], [modT.b])
            V(lambda: v.tensor_scalar_add(out=modT[:, 16:32, :], in0=modT[:, 16:32, :], scalar1=1.0), [modT.b], [modT.b])
            V(lambda: v.tensor_scalar_add(out=modT[:, 64:80, :], in0=modT[:, 64:80, :], scalar1=1.0), [modT.b], [modT.b])
            k.barrier()

        def ln_stats(es_tmp, xt, xb, mv, rstd):
            st = es_tmp
            for i in range(4):
                V(lambda i=i: v.bn_stats(out=st[:, i * 6:(i + 1) * 6], in_=xt[:, i * 512:(i + 1) * 512]), [xb], [st.b])
            V(lambda: v.bn_aggr(out=mv[:, 0:2], in_=st[:, 0:24]), [st.b], [mv.b])
            A(lambda: a.activation(out=rstd[:, 0:1], in_=mv[:, 1:2], func=AF.Sqrt, bias=eps_t[:, 0:1], scale=1.0), [mv.b, eps_t.b], [rstd.b])
            V(lambda: v.reciprocal(out=rstd[:, 0:1], in_=rstd[:, 0:1]), [rstd.b], [rstd.b])

        def normalize_T(xt, xb, xn, st, mv, rstd, cv, sh_c, sc_c, outs):
            ln_stats(st, xt, xb, mv, rstd)
            V(lambda: v.tensor_scalar(out=xn[:, :], in0=xt, scalar1=mv[:, 0:1], scalar2=rstd[:, 0:1],
                                      op0=ALU.subtract, op1=ALU.mult), [xb, mv.b, rstd.b], [xn.b])
            for c4 in range(4):
                ps = k.psum()

                def tr(ps=ps, c4=c4):
                    for j in range(4):
                        c = c4 * 4 + j
                        r = pe.transpose(ps[:, j * 128:(j + 1) * 128], xn[:, c * 128:(c + 1) * 128], ident)
                    return r
                T(tr, [xn.b, cst.b], [ps.b])
                for j in range(4):
                    c = c4 * 4 + j
                    for (apf, ob) in outs:
                        if (c + (0 if len(outs) == 1 else 0)) % 2 == 0:
                            V(lambda ps=ps, j=j, c=c, apf=apf: v.tensor_scalar(
                                out=apf(c), in0=ps[:, j * 128:(j + 1) * 128], scalar1=modT[:, sc_c + c, cv:cv + 1],
                                scalar2=modT[:, sh_c + c, cv:cv + 1], op0=ALU.mult, op1=ALU.add), [ps.b, modT.b], [ob])
                        else:
                            A(lambda ps=ps, j=j, c=c, apf=apf: a.activation(
                                out=apf(c), in_=ps[:, j * 128:(j + 1) * 128], func=AF.Identity,
                                bias=modT[:, sh_c + c, cv:cv + 1], scale=modT[:, sc_c + c, cv:cv + 1]), [ps.b, modT.b], [ob])

        winv = win_d.rearrange("(kc p) n -> p kc n", p=128)

        def unit(es, u, sample):
            TS = 1024 if sample else 256
            NCH = TS // 128
            cv = 1 if sample else 0
            row0 = NPT * 128 if sample else u * 256
            xown_d = xs_own_d if sample else xp_d[u * 256:(u + 1) * 256, :]
            xseq_d = xs_seq_d if sample else xown_d
            st = sb(es, "st", [128, 24])
            mv = sb(es, "mv", [128, 2])
            rstd = sb(es, "rstd", [128, 1])
            NWT = 2 if sample else 4
            wt = [sb(es, "wt%d" % i, [128, 16, 256], BF16) for i in range(NWT)]
            wti = [0]
            ynT = sb(es, "ynT", [128, 16, 256], BF16)
            ysT = sb(es, "ysT", [128, 16, 256], BF16)
            gsT = sb(es, "gsT", [128, 16, 256], BF16)
            gnT = sb(es, "gnT", [128, 16, 256], BF16)
            hT = sb(es, "hT", [128, 16, TS], BF16)
            hTo = sb(es, "hTo", [128, 16, 256], BF16) if sample else hT

            def wtile(dram_view, c0, ncols):
                w = wt[wti[0] % NWT]
                wti[0] += 1
                k.dma(k.pool, w[:, :, 0:ncols], dram_view[:, :, c0:c0 + ncols], writes=[w.b], max_dma_last_dim=2048)
                return w

            def proj_fm(w, wc, src, tok0, ntok, ps):
                def mm():
                    for kc in range(16):
                        r = pe.matmul(ps[:, 0:ntok], w[:, kc, wc * 128:(wc + 1) * 128], src[:, kc, tok0:tok0 + ntok],
                                      start=(kc == 0), stop=(kc == 15))
                    return r
                T(mm, [w.b, src.b], [ps.b])

            def proj_tm(w, ncols, src, tt, ps):
                def mm():
                    for kc in range(16):
                        r = pe.matmul(ps[:, 0:ncols], src[:, kc, tt * 128:(tt + 1) * 128], w[:, kc, 0:ncols],
                                      start=(kc == 0), stop=(kc == 15))
                    return r
                T(mm, [w.b, src.b], [ps.b])

            with contextlib.ExitStack() as e1:
                xq = sb(e1, "xq", [128, D])
                xn = sb(e1, "xn", [128, D])
                if sample:
                    for tt in range(NCH):
                        k.dma(k.sp, xq[:, :], xseq_d[tt * 128:(tt + 1) * 128, :], writes=[xq.b])
                        normalize_T(xq[:, :], xq.b, xn, st, mv, rstd, cv, 0, 16,
                                    [(lambda c, tt=tt: hT[:, c, tt * 128:(tt + 1) * 128], hT.b)])
                for tt in range(2):
                    k.dma(k.sp, xq[:, :], xown_d[tt * 128:(tt + 1) * 128, :], writes=[xq.b])
                    normalize_T(xq[:, :], xq.b, xn, st, mv, rstd, cv, 0, 16,
                                [(lambda c, tt=tt: hTo[:, c, tt * 128:(tt + 1) * 128], hTo.b)])
            k.barrier()

            if LIM <= 2:
                return
            for (c0, gt) in ((C_GS, gsT), (C_GN, gnT)):
                for ct in range(8):
                    w = wtile(winv, c0 + ct * 256, 256)
                    for wc in range(2):
                        ps = k.psum()
                        proj_fm(w, wc, hTo, 0, 256, ps)
                        A(lambda ps=ps, gt=gt, cc=ct * 2 + wc: a.activation(out=gt[:, cc, :], in_=ps[:, 0:256], func=AF.Sigmoid), [ps.b], [gt.b])

            if LIM <= 3:
                return
            with contextlib.ExitStack() as ea:
                qT = sb(ea, "qT", [128, 16, 256], BF16)
                kT = sb(ea, "kT", [128, 16, TS], BF16)
                vb = sb(ea, "vb", [128, NCH, D], BF16)
                stg = [sb(ea, "stg%d" % i, [128, 256]) for i in range(2)] if not sample else None
                stgi = [0]
                SC = 128.0 ** -0.5
                for ct in range(8):
                    w = wtile(winv, C_Q + ct * 256, 256)
                    for wc in range(2):
                        ps = k.psum()
                        proj_fm(w, wc, hTo, 0, 256, ps)
                        A(lambda ps=ps, h=ct * 2 + wc: a.activation(out=qT[:, h, :], in_=ps[:, 0:256], func=AF.Identity, scale=SC), [ps.b], [qT.b])
                if LIM <= 3.1:
                    return
                for ct in range(8):
                    w = wtile(winv, C_K + ct * 256, 256)
                    for wc in range(2):
                        for t0 in range(0, TS, 512):
                            n = min(512, TS - t0)
                            ps = k.psum()
                            proj_fm(w, wc, hT, t0, n, ps)
                            V(lambda ps=ps, h=ct * 2 + wc, t0=t0, n=n: v.tensor_copy(out=kT[:, h, t0:t0 + n], in_=ps[:, 0:n]), [ps.b], [kT.b])
                    if not sample:
                        for tt in range(2):
                            ps = k.psum()
                            proj_tm(w, 256, hT, tt, ps)
                            s = stg[stgi[0] % 2]
                            stgi[0] += 1
                            A(lambda ps=ps, s=s: a.activation(func=AF.Identity, out=s[:, :], in_=ps[:, 0:256]), [ps.b], [s.b])
                            if LIM != 3.3:
                                k.dma(k.sp, nk_o[u * 256 + tt * 128:u * 256 + (tt + 1) * 128, ct * 256:(ct + 1) * 256], s[:, :], reads=[s.b])
                if LIM <= 3.2:
                    return
                for ct in range(8):
                    w = wtile(winv, C_V + ct * 256, 256)
                    for tt in range(NCH):
                        ps = k.psum()
                        proj_tm(w, 256, hT, tt, ps)
                        A(lambda ps=ps, tt=tt, ct=ct: a.activation(func=AF.Identity, out=vb[:, tt, ct * 256:(ct + 1) * 256], in_=ps[:, 0:256]), [ps.b], [vb.b])
                        if not sample:
                            s = stg[stgi[0] % 2]
                            stgi[0] += 1
                            A(lambda ps=ps, s=s: a.activation(func=AF.Identity, out=s[:, :], in_=ps[:, 0:256]), [ps.b], [s.b])
                            if LIM != 3.3:
                                k.dma(k.sp, nv_o[u * 256 + tt * 128:u * 256 + (tt + 1) * 128, ct * 256:(ct + 1) * 256], s[:, :], reads=[s.b])
                if LIM <= 3.3:
                    return
                NKB = (TS + (512 if sample else 0)) // 128
                NKEY = NKB * 128
                NB = 2
                S_l = [sb(ea, "S%d" % i, [128, NKEY]) for i in range(NB)]
                PT_l = [sb(ea, "PT%d" % i, [128, NKB, 128], BF16) for i in range(NB)]
                mx_l = [sb(ea, "mx%d" % i, [128, 2]) for i in range(NB)]
                rs_l = [sb(ea, "rs%d" % i, [128, 2]) for i in range(NB)]
                if sample:
                    kc_l = [sb(ea, "kctx%d" % i, [128, 512], BF16) for i in range(2)]
                    vc_l = [sb(ea, "vctx%d" % i, [128, 4, 128], BF16) for i in range(2)]
                    bia_l = [sb(ea, "bia%d" % i, [128, 1024]) for i in range(2)]
                    vctxv = vctx_d.rearrange("(t p) n -> p t n", p=128)

                def stage1(it):
                    h, qt = divmod(it, 2)
                    S, mx, rs = S_l[it % NB], mx_l[it % NB], rs_l[it % NB]
                    if sample:
                        kc_t, bia = kc_l[h % 2], bia_l[it % 2]
                        if qt == 0:
                            k.dma(k.pool, kc_t[:, :], kctxT_d[h, :, :], writes=[kc_t.b], max_dma_last_dim=2048)
                            k.dma(k.pool, vc_l[h % 2][:, :, :], vctxv[:, :, h * 128:(h + 1) * 128], writes=[vc_l[h % 2].b])
                        k.dma(k.sp, bia[:, :], bias_d[h, qt * 128:(qt + 1) * 128, :], writes=[bia.b])
                    for kb in range(0, NKEY, 512):
                        n = min(512, NKEY - kb)
                        ps = k.psum()
                        if kb < TS:
                            T(lambda ps=ps, kb=kb, n=n: pe.matmul(ps[:, 0:n], qT[:, h, qt * 128:(qt + 1) * 128], kT[:, h, kb:kb + n], start=True, stop=True),
                              [qT.b, kT.b], [ps.b])
                            if sample:
                                V(lambda ps=ps, kb=kb, n=n: v.tensor_tensor(out=S[:, kb:kb + n], in0=ps[:, 0:n], in1=bia[:, kb:kb + n], op=ALU.add), [ps.b, bia.b], [S.b])
                            else:
                                V(lambda ps=ps, kb=kb, n=n: v.tensor_copy(out=S[:, kb:kb + n], in_=ps[:, 0:n]), [ps.b], [S.b])
                        else:
                            T(lambda ps=ps, n=n: pe.matmul(ps[:, 0:n], qT[:, h, qt * 128:(qt + 1) * 128], kc_t[:, 0:n], start=True, stop=True),
                              [qT.b, kc_t.b], [ps.b])
                            A(lambda ps=ps, kb=kb, n=n: a.activation(func=AF.Identity, out=S[:, kb:kb + n], in_=ps[:, 0:n]), [ps.b], [S.b])
                    V(lambda: v.reduce_max(out=mx[:, 0:1], in_=S[:, :], axis=AX.X), [S.b], [mx.b])
                    V(lambda: v.tensor_scalar(out=mx[:, 1:2], in0=mx[:, 0:1], scalar1=-1.0, scalar2=None, op0=ALU.mult), [mx.b], [mx.b])
                    A(lambda: a.activation(out=S[:, :], in_=S[:, :], func=AF.Exp, bias=mx[:, 1:2], scale=1.0, accum_out=rs[:, 0:1]), [S.b, mx.b], [S.b, rs.b])
                    V(lambda: v.reciprocal(out=rs[:, 1:2], in_=rs[:, 0:1]), [rs.b], [rs.b])
                    V(lambda: v.tensor_scalar(out=S[:, :], in0=S[:, :], scalar1=rs[:, 1:2], scalar2=None, op0=ALU.mult), [S.b, rs.b], [S.b])

                def stage2(it):
                    h, qt = divmod(it, 2)
                    S, PT = S_l[it % NB], PT_l[it % NB]
                    for k4 in range(0, NKB, 4):
                        ps = k.psum()
                        nn = min(4, NKB - k4)

                        def tr(ps=ps, k4=k4, nn=nn):
                            for j in range(nn):
                                r = pe.transpose(ps[:, j * 128:(j + 1) * 128], S[:, (k4 + j) * 128:(k4 + j + 1) * 128], ident)
                            return r
                        T(tr, [S.b, cst.b], [ps.b])
                        if (k4 // 4) % 2 == 0:
                            V(lambda ps=ps, k4=k4, nn=nn: v.tensor_copy(out=PT[:, k4:k4 + nn, :], in_=ps[:, 0:nn * 128].rearrange("p (j q) -> p j q", q=128)), [ps.b], [PT.b])
                        else:
                            A(lambda ps=ps, k4=k4, nn=nn: a.activation(func=AF.Identity, out=PT[:, k4:k4 + nn, :], in_=ps[:, 0:nn * 128].rearrange("p (j q) -> p j q", q=128)), [ps.b], [PT.b])
                    ps = k.psum()
                    vc_t = vc_l[h % 2] if sample else None

                    def pv(ps=ps):
                        for kb in range(NKB):
                            if kb < NCH:
                                lhs = vb[:, kb, h * 128:(h + 1) * 128]
                            else:
                                lhs = vc_t[:, kb - NCH, :]
                            r = pe.matmul(ps[:, 0:128], lhs, PT[:, kb, :], start=(kb == 0), stop=(kb == NKB - 1))
                        return r
                    T(pv, [vb.b, PT.b] + ([vc_t.b] if sample else []), [ps.b])
                    A(lambda ps=ps: a.activation(func=AF.Identity, out=ynT[:, h, qt * 128:(qt + 1) * 128], in_=ps[:, 0:128]), [ps.b], [ynT.b])

                stage1(0)
                for it in range(32):
                    if it + 1 < 32:
                        stage1(it + 1)
                    stage2(it)
                k.barrier()

            if LIM <= 4:
                return
            with contextlib.ExitStack() as e2:
                xtm = sb(e2, "xtm", [128, NCH, D], BF16)
                Btm = sb(e2, "Btm", [128, NCH, 512], BF16)
                BT = sb(e2, "BT", [128, 4, TS], BF16)
                CT = sb(e2, "CT", [128, 4, TS], BF16)
                dtt = sb(e2, "dtt", [128, NCH, 64])
                dAt = sb(e2, "dAt", [128, NCH, 64])
                yown = sb(e2, "yown", [128, 2, D])
                cw = sb(e2, "cw", [128, 24, 5])
                cb = sb(e2, "cb", [128, 24])
                dtb = sb(e2, "dtb", [128, 64])
                Abc = sb(e2, "Abc", [128, 64])
                dbc = sb(e2, "dbc", [128, 32])
                nw = sb(e2, "nw", [128, 16])
                k.dma(k.sp, cw[:, :, :], convw_d[:, :, :], writes=[cw.b])
                k.dma(k.sp, cb[:, :], convb_d[:, :], writes=[cb.b])
                k.dma(k.sp, dtb[:, :], dtb_d[0:1, :].to_broadcast([128, 64]), writes=[dtb.b])
                k.dma(k.sp, Abc[:, :], alog_d[0:1, :].to_broadcast([128, 64]), writes=[Abc.b])
                k.dma(k.sp, dbc[:, :], ssdd_d[0:1, :].to_broadcast([128, 32]), writes=[dbc.b])
                k.dma(k.sp, nw[:, :], normw_d[:, :], writes=[nw.b])
                A(lambda: a.activation(out=Abc[:, :], in_=Abc[:, :], func=AF.Exp), [Abc.b], [Abc.b])
                V(lambda: v.tensor_scalar(out=Abc[:, :], in0=Abc[:, :], scalar1=-1.0, scalar2=None, op0=ALU.mult), [Abc.b], [Abc.b])
                if sample:
                    sel = sb(e2, "sel", [128, 16])
                    k.dma(k.sp, sel[:, :], sel_d[:, :], writes=[sel.b])
                with contextlib.ExitStack() as e3:
                    xpad = [sb(e3, "xpad%d" % i, [128, TS + 4]) for i in range(2)]
                    cacc = [sb(e3, "cacc%d" % i, [128, TS]) for i in range(2)]
                    for xp_ in xpad:
                        V(lambda xp_=xp_: v.memset(xp_[:, :], 0.0), writes=[xp_.b])
                    for ct in range(12):
                        w = wtile(winv, C_XBC + ct * 256, 256)
                        for wc in range(2):
                            c = ct * 2 + wc
                            xp_ = xpad[c % 2]
                            ca = cacc[c % 2]
                            for t0 in range(0, TS, 512):
                                n = min(512, TS - t0)
                                ps = k.psum()
                                proj_fm(w, wc, hT, t0, n, ps)
                                A(lambda ps=ps, xp_=xp_, t0=t0, n=n: a.activation(func=AF.Identity, out=xp_[:, 2 + t0:2 + t0 + n], in_=ps[:, 0:n]), [ps.b], [xp_.b])
                            V(lambda xp_=xp_, ca=ca, c=c: v.tensor_scalar(out=ca[:, :], in0=xp_[:, 0:TS], scalar1=cw[:, c, 0:1], scalar2=None, op0=ALU.mult),
                              [xp_.b, cw.b], [ca.b])
                            for j in range(1, 5):
                                V(lambda xp_=xp_, ca=ca, c=c, j=j: v.scalar_tensor_tensor(out=ca[:, :], in0=xp_[:, j:j + TS], scalar=cw[:, c, j:j + 1], in1=ca[:, :],
                                                                                         op0=ALU.mult, op1=ALU.add), [xp_.b, cw.b, ca.b], [ca.b])
                            if c < 20:
                                A(lambda ca=ca, c=c: a.activation(out=ca[:, :], in_=ca[:, :], func=AF.Silu, bias=cb[:, c:c + 1], scale=1.0), [ca.b, cb.b], [ca.b])
                                if c >= 16:
                                    V(lambda ca=ca, c=c: v.tensor_copy(out=BT[:, c - 16, :], in_=ca[:, :]), [ca.b], [BT.b])
                                for t4 in range(0, NCH, 4):
                                    nn = min(4, NCH - t4)
                                    ps = k.psum()

                                    def tr(ps=ps, t4=t4, nn=nn, ca=ca):
                                        for j in range(nn):
                                            r = pe.transpose(ps[:, j * 128:(j + 1) * 128], ca[:, (t4 + j) * 128:(t4 + j + 1) * 128], ident)
                                        return r
                                    T(tr, [ca.b, cst.b], [ps.b])
                                    dst = xtm if c < 16 else Btm
                                    col = c * 128 if c < 16 else (c - 16) * 128
                                    V(lambda ps=ps, t4=t4, nn=nn, dst=dst, col=col: v.tensor_copy(
                                        out=dst[:, t4:t4 + nn, col:col + 128], in_=ps[:, 0:nn * 128].rearrange("p (j q) -> p j q", q=128)), [ps.b], [dst.b])
                            else:
                                A(lambda ca=ca, c=c: a.activation(out=CT[:, c - 20, :], in_=ca[:, :], func=AF.Silu, bias=cb[:, c:c + 1], scale=1.0), [ca.b, cb.b], [CT.b])
                    w = wtile(winv, C_DT, 64)
                    t1 = sb(e3, "t1", [128, 64])
                    t2 = sb(e3, "t2", [128, 64])
                    for tt in range(NCH):
                        ps = k.psum()
                        proj_tm(w, 64, hT, tt, ps)
                        V(lambda ps=ps: v.tensor_tensor(out=t1[:, :], in0=ps[:, 0:64], in1=dtb[:, :], op=ALU.add), [ps.b, dtb.b], [t1.b])
                        A(lambda: a.activation(out=t2[:, :], in_=t1[:, :], func=AF.Abs), [t1.b], [t2.b])
                        A(lambda: a.activation(out=t2[:, :], in_=t2[:, :], func=AF.Exp, scale=-1.0), [t2.b], [t2.b])
                        A(lambda: a.activation(out=t2[:, :], in_=t2[:, :], func=AF.Ln, bias=eps_t[:, 1:2], scale=1.0), [t2.b, eps_t.b], [t2.b])
                        V(lambda tt=tt: v.scalar_tensor_tensor(out=dtt[:, tt, :], in0=t1[:, :], scalar=0.0, in1=t2[:, :], op0=ALU.max, op1=ALU.add), [t1.b, t2.b], [dtt.b])
                        V(lambda tt=tt: v.tensor_tensor(out=dAt[:, tt, :], in0=dtt[:, tt, :], in1=Abc[:, :], op=ALU.mult), [dtt.b, Abc.b], [dAt.b])
                    k.barrier()
                if LIM <= 5:
                    return
                if sample:
                    V(lambda: v.memset(yown[:, :, :], 0.0), writes=[yown.b])
                for d in range(2):
                    with contextlib.ExitStack() as e3:
                        PB = 1 if sample else 2
                        STd = sb(e3, "ST", [128, D])
                        STbd = sb(e3, "STb", [128, D], BF16)
                        CBm_l = [sb(e3, "CBm%d" % i, [128, 4, 128]) for i in range(PB)]
                        nacs_l = [sb(e3, "nacs%d" % i, [128, 32]) for i in range(PB)]
                        eacs_l = [sb(e3, "eacs%d" % i, [128, 32]) for i in range(PB)]
                        etot_l = [sb(e3, "etot%d" % i, [128, 32]) for i in range(PB)]
                        coef_l = [sb(e3, "coef%d" % i, [128, 32]) for i in range(PB)]
                        xdt_l = [sb(e3, "xdt%d" % i, [128, D], BF16) for i in range(PB)]
                        xdd_l = [sb(e3, "xdd%d" % i, [128, D], BF16) for i in range(PB)]
                        E4 = [sb(e3, "E4_%d" % i, [128, 512]) for i in range(2)]
                        G4 = [sb(e3, "G4_%d" % i, [128, 512], BF16) for i in range(2)]
                        E4b = [[Buf() for _ in range(4)] for _ in range(2)]
                        gi4 = [0]
                        ytmp = sb(e3, "ytmp", [128, 512])
                        ytmp2 = sb(e3, "ytmp2", [128, 512])
                        if sample:
                            k.dma(k.sp, STd[:, :], s0_d[d, :, :], writes=[STd.b])
                        else:
                            V(lambda: v.memset(STd[:, :], 0.0), writes=[STd.b])
                        V(lambda: v.tensor_copy(out=STbd[:, :], in_=STd[:, :]), [STd.b], [STbd.b])
                        tri = trif if d == 0 else trib
                        groups = [(g, h4) for g in range(4) for h4 in range(2)]

                        def prologue(c, slot):
                            nacs, eacs, etot, coef = nacs_l[slot], eacs_l[slot], etot_l[slot], coef_l[slot]
                            xdt, xdd, CBm = xdt_l[slot], xdd_l[slot], CBm_l[slot]
                            tok = slice(c * 128, (c + 1) * 128)
                            psA = k.psum()
                            T(lambda: pe.matmul(psA[:, 0:32], tri, dAt[:, c, d * 32:(d + 1) * 32], start=True, stop=True),
                              [cst.b, dAt.b], [psA.b])
                            psT = k.psum()
                            T(lambda: pe.matmul(psT[:, 0:32], ones, dAt[:, c, d * 32:(d + 1) * 32], start=True, stop=True),
                              [cst.b, dAt.b], [psT.b])
                            V(lambda: v.tensor_scalar(out=nacs[:, :], in0=psA[:, 0:32], scalar1=-1.0, scalar2=None, op0=ALU.mult), [psA.b], [nacs.b])
                            A(lambda: a.activation(out=eacs[:, :], in_=psA[:, 0:32], func=AF.Exp), [psA.b], [eacs.b])
                            A(lambda: a.activation(out=etot[:, :], in_=psT[:, 0:32], func=AF.Exp), [psT.b], [etot.b])
                            V(lambda: v.tensor_tensor(out=coef[:, :], in0=psT[:, 0:32], in1=nacs[:, :], op=ALU.add), [psT.b, nacs.b], [coef.b])
                            A(lambda: a.activation(out=coef[:, :], in_=coef[:, :], func=AF.Exp), [coef.b], [coef.b])
                            V(lambda: v.tensor_tensor(out=coef[:, :], in0=coef[:, :], in1=dtt[:, c, d * 32:(d + 1) * 32], op=ALU.mult), [coef.b, dtt.b], [coef.b])
                            xv = xtm[:, c, :].rearrange("p (h q) -> p h q", q=64)
                            V(lambda: v.tensor_tensor(out=xdt[:, :].rearrange("p (h q) -> p h q", q=64), in0=xv,
                                                      in1=bc_last(dtt[:, c, d * 32:(d + 1) * 32], 64), op=ALU.mult), [xtm.b, dtt.b], [xdt.b])
                            V(lambda: v.tensor_tensor(out=xdd[:, :].rearrange("p (h q) -> p h q", q=64), in0=xv,
                                                      in1=bc_last(coef[:, :], 64), op=ALU.mult), [xtm.b, coef.b], [xdd.b])
                            psC = k.psum()

                            def cbm():
                                for g in range(4):
                                    r = pe.matmul(psC[:, g * 128:(g + 1) * 128], BT[:, g, tok], CT[:, g, tok], start=True, stop=True)
                                return r
                            T(cbm, [BT.b, CT.b], [psC.b])
                            V(lambda: v.tensor_tensor(out=CBm[:, :, :], in0=psC[:, :].rearrange("p (g i) -> p g i", i=128),
                                                      in1=bc_mid(tri, 4), op=ALU.mult), [psC.b, cst.b], [CBm.b])
                            return (nacs, eacs, etot, xdt, xdd, CBm)

                        def body(c, P):
                            nacs, eacs, etot, xdt, xdd, CBm = P
                            tok = slice(c * 128, (c + 1) * 128)
                            psRs = {}

                            def emit_rr(gi):
                                g, h4 = groups[gi]
                                psR = k.psum()

                                def rr():
                                    for j in range(4):
                                        hh = d * 32 + g * 8 + h4 * 4 + j
                                        r = pe.matmul(psR[:, j * 128:(j + 1) * 128], dAt[:, c, hh:hh + 1].to_broadcast([128, 128]), tri, start=True, stop=True)
                                    return r
                                T(rr, [dAt.b, cst.b], [psR.b])
                                psRs[gi] = psR
                            emit_rr(0)
                            for gi, (g, h4) in enumerate(groups):
                                if h4 == 0:
                                    psY, psO, psS = k.psum(), k.psum(), k.psum()
                                if gi + 1 < len(groups):
                                    emit_rr(gi + 1)
                                psR = psRs.pop(gi)
                                i2 = gi4[0] % 2
                                gi4[0] += 1
                                E, G, Eb = E4[i2], G4[i2], E4b[i2]
                                for j in range(4):
                                    hl = g * 8 + h4 * 4 + j
                                    A(lambda psR=psR, j=j, hl=hl, E=E: a.activation(out=E[:, j * 128:(j + 1) * 128], in_=psR[:, j * 128:(j + 1) * 128], func=AF.Exp,
                                                                                  bias=nacs[:, hl:hl + 1], scale=1.0), [psR.b, nacs.b], [Eb[j]])
                                V(lambda E=E, G=G, g=g: v.scalar_tensor_tensor(out=G[:, :].rearrange("p (j q) -> p j q", q=128), in0=E[:, :].rearrange("p (j q) -> p j q", q=128),
                                                                               scalar=1.0, in1=bc_mid(CBm[:, g, :], 4), op0=ALU.min, op1=ALU.mult),
                                  Eb + [CBm.b], [G.b])
                                for j in range(4):
                                    hl = g * 8 + h4 * 4 + j
                                    o = (h4 * 4 + j) * 64
                                    T(lambda psO=psO, g=g, hl=hl, o=o: pe.matmul(psO[:, o:o + 64], CT[:, g, tok], STbd[:, hl * 64:(hl + 1) * 64], start=True, stop=True),
                                      [CT.b, STbd.b], [psO.b])
                                    T(lambda psS=psS, g=g, hl=hl, o=o: pe.matmul(psS[:, o:o + 64], Btm[:, c, g * 128:(g + 1) * 128], xdd[:, hl * 64:(hl + 1) * 64], start=True, stop=True),
                                      [Btm.b, xdd.b], [psS.b])
                                for j in range(4):
                                    hl = g * 8 + h4 * 4 + j
                                    o = (h4 * 4 + j) * 64
                                    T(lambda psY=psY, G=G, j=j, hl=hl, o=o: pe.matmul(psY[:, o:o + 64], G[:, j * 128:(j + 1) * 128], xdt[:, hl * 64:(hl + 1) * 64], start=True, stop=True),
                                      [G.b, xdt.b], [psY.b])
                                if h4 == 0:
                                    continue
                                gs = slice(g * 512, (g + 1) * 512)
                                V(lambda psO=psO, g=g: v.tensor_tensor(out=ytmp[:, :].rearrange("p (h q) -> p h q", q=64), in0=psO[:, :].rearrange("p (h q) -> p h q", q=64),
                                                                      in1=bc_last(eacs[:, g * 8:(g + 1) * 8], 64), op=ALU.mult), [psO.b, eacs.b], [ytmp.b])
                                if sample:
                                    V(lambda psY=psY: v.tensor_tensor(out=ytmp[:, :], in0=ytmp[:, :], in1=psY[:, :], op=ALU.add), [ytmp.b, psY.b], [ytmp.b])
                                    for sl in range(2):
                                        V(lambda sl=sl, gs=gs: v.scalar_tensor_tensor(out=yown[:, sl, gs], in0=ytmp[:, :], scalar=sel[:, c * 2 + sl:c * 2 + sl + 1],
                                                                                       in1=yown[:, sl, gs], op0=ALU.mult, op1=ALU.add), [ytmp.b, sel.b, yown.b], [yown.b])
                                else:
                                    if d == 0:
                                        V(lambda psY=psY, gs=gs: v.tensor_tensor(out=yown[:, c, gs], in0=ytmp[:, :], in1=psY[:, :], op=ALU.add), [ytmp.b, psY.b], [yown.b])
                                    else:
                                        V(lambda psY=psY: v.tensor_tensor(out=ytmp[:, :], in0=ytmp[:, :], in1=psY[:, :], op=ALU.add), [ytmp.b, psY.b], [ytmp.b])
                                        V(lambda gs=gs: v.tensor_tensor(out=yown[:, c, gs], in0=yown[:, c, gs], in1=ytmp[:, :], op=ALU.add), [ytmp.b, yown.b], [yown.b])
                                V(lambda g=g, gs=gs: v.tensor_tensor(out=ytmp2[:, :].rearrange("p (h q) -> p h q", q=64), in0=STd[:, gs].rearrange("p (h q) -> p h q", q=64),
                                                                     in1=bc_last(etot[:, g * 8:(g + 1) * 8], 64), op=ALU.mult), [STd.b, etot.b], [ytmp2.b])
                                V(lambda psS=psS, gs=gs: v.tensor_tensor(out=STd[:, gs], in0=ytmp2[:, :], in1=psS[:, :], op=ALU.add), [ytmp2.b, psS.b], [STd.b])
                            A(lambda: a.activation(func=AF.Identity, out=STbd[:, :], in_=STd[:, :]), [STd.b], [STbd.b])

                        order = list(range(NCH)) if d == 0 else list(range(NCH - 1, -1, -1))
                        if PB == 2:
                            P = prologue(order[0], 0)
                            for idx, c in enumerate(order):
                                Pn = prologue(order[idx + 1], (idx + 1) % 2) if idx + 1 < len(order) else None
                                body(c, P)
                                P = Pn
                        else:
                            for c in order:
                                body(c, prologue(c, 0))
                        if not sample:
                            so = sb(e3, "so", [128, 16, 128])
                            for c4 in range(4):
                                ps = k.psum()

                                def tr(ps=ps, c4=c4):
                                    for j in range(4):
                                        cc = c4 * 4 + j
                                        r = pe.transpose(ps[:, j * 128:(j + 1) * 128], STd[:, cc * 128:(cc + 1) * 128], ident)
                                    return r
                                T(tr, [STd.b, cst.b], [ps.b])
                                V(lambda ps=ps, c4=c4: v.tensor_copy(out=so[:, c4 * 4:(c4 + 1) * 4, :], in_=ps[:, :].rearrange("p (j q) -> p j q", q=128)), [ps.b], [so.b])
                            dst = (hf_o if d == 0 else hb_o)[u].rearrange("(c p) n -> p c n", p=128)
                            k.dma(k.sp, dst, so[:, :, :], reads=[so.b])
                        k.barrier()
                if LIM <= 6:
                    return
                with contextlib.ExitStack() as e3:
                    zs = sb(e3, "zs", [128, 2, D], BF16)
                    xn = sb(e3, "xnz", [128, D])
                    xso = sb(e3, "xso", [128, D])
                    ss = sb(e3, "ss", [128, 2])
                    for ct in range(8):
                        w = wtile(winv, C_Z + ct * 256, 256)
                        for tt in range(2):
                            ps = k.psum()
                            proj_tm(w, 256, hTo, tt, ps)
                            A(lambda ps=ps, tt=tt, ct=ct: a.activation(out=zs[:, tt, ct * 256:(ct + 1) * 256], in_=ps[:, 0:256], func=AF.Silu), [ps.b], [zs.b])
                    for tt in range(2):
                        if sample:
                            V(lambda: v.memset(xso[:, :], 0.0), writes=[xso.b])
                            for c in range(NCH):
                                V(lambda c=c, tt=tt: v.scalar_tensor_tensor(out=xso[:, :], in0=xtm[:, c, :], scalar=sel[:, c * 2 + tt:c * 2 + tt + 1], in1=xso[:, :],
                                                                            op0=ALU.mult, op1=ALU.add), [xtm.b, sel.b, xso.b], [xso.b])
                            xsrc = xso[:, :]
                            xsb = xso.b
                        else:
                            xsrc = xtm[:, tt, :]
                            xsb = xtm.b
                        V(lambda xsrc=xsrc: v.tensor_tensor(out=xn[:, :].rearrange("p (h q) -> p h q", q=64), in0=xsrc.rearrange("p (h q) -> p h q", q=64),
                                                            in1=bc_last(dbc[:, :], 64), op=ALU.mult), [xsb, dbc.b], [xn.b])
                        V(lambda tt=tt: v.tensor_tensor(out=xn[:, :], in0=xn[:, :], in1=yown[:, tt, :], op=ALU.add), [xn.b, yown.b], [xn.b])
                        V(lambda tt=tt: v.tensor_tensor(out=xn[:, :], in0=xn[:, :], in1=zs[:, tt, :], op=ALU.mult), [xn.b, zs.b], [xn.b])
                        A(lambda tt=tt: a.activation(out=yown[:, tt, :], in_=xn[:, :], func=AF.Square, accum_out=ss[:, 0:1]), [xn.b], [yown.b, ss.b])
                        A(lambda: a.activation(out=ss[:, 1:2], in_=ss[:, 0:1], func=AF.Sqrt, bias=eps_t[:, 0:1], scale=1.0 / D), [ss.b, eps_t.b], [ss.b])
                        V(lambda: v.reciprocal(out=ss[:, 1:2], in_=ss[:, 1:2]), [ss.b], [ss.b])
                        V(lambda: v.tensor_scalar(out=xn[:, :], in0=xn[:, :], scalar1=ss[:, 1:2], scalar2=None, op0=ALU.mult), [xn.b, ss.b], [xn.b])
                        for c4 in range(4):
                            ps = k.psum()

                            def tr(ps=ps, c4=c4):
                                for j in range(4):
                                    cc = c4 * 4 + j
                                    r = pe.transpose(ps[:, j * 128:(j + 1) * 128], xn[:, cc * 128:(cc + 1) * 128], ident)
                                return r
                            T(tr, [xn.b, cst.b], [ps.b])
                            for j in range(4):
                                cc = c4 * 4 + j
                                V(lambda ps=ps, j=j, cc=cc, tt=tt: v.tensor_scalar(out=ysT[:, cc, tt * 128:(tt + 1) * 128], in0=ps[:, j * 128:(j + 1) * 128],
                                                                                   scalar1=nw[:, cc:cc + 1], scalar2=None, op0=ALU.mult), [ps.b, nw.b], [ysT.b])
                k.barrier()
            if LIM <= 7:
                return
            if DBG and sample:
                for nm, tns in (("yn", ynT), ("ys", ysT), ("gs", gsT), ("gn", gnT)):
                    k.dma(k.pool, dbg[nm][:, :, :], tns[:, :, :], reads=[tns.b])
            with contextlib.ExitStack() as e3:
                xo = sb(e3, "xo", [128, 2, D])
                xn = sb(e3, "xnm", [128, D])
                m2bc = sb(e3, "m2bc", [128, D])
                lnbc = sb(e3, "lnbc", [128, 2, D])
                mixT = sb(e3, "mixT", [128, 16, 256], BF16)
                m1 = sb(e3, "m1", [128, 256])
                m2 = sb(e3, "m2", [128, 256])
                uu = sb(e3, "uu", [128, 2, D])
                tmp = sb(e3, "tmpo", [128, 256])
                for tt in range(2):
                    k.dma(k.sp, xo[:, tt, :], xown_d[tt * 128:(tt + 1) * 128, :], writes=[xo.b])
                k.dma(k.sp, m2bc[:, :], mods_d[cv:cv + 1, 2 * D:3 * D].to_broadcast([128, D]), reads=[modsd_b], writes=[m2bc.b])
                k.dma(k.sp, lnbc[:, 0, :], lnv_d[0:1, :].to_broadcast([128, D]), writes=[lnbc.b])
                k.dma(k.sp, lnbc[:, 1, :], lnv_d[1:2, :].to_broadcast([128, D]), writes=[lnbc.b])
                wnav = wna_d.rearrange("(kc p) n -> p kc n", p=128)
                wssv = wss_d.rearrange("(kc p) n -> p kc n", p=128)
                wov = wo_d.rearrange("(kc p) n -> p kc n", p=128)
                for ct in range(8):
                    wa = wtile(wnav, ct * 256, 256)
                    wb_ = wtile(wssv, ct * 256, 256)
                    for wc in range(2):
                        cc = ct * 2 + wc
                        ps1, ps2 = k.psum(), k.psum()
                        proj_fm(wa, wc, ynT, 0, 256, ps1)
                        proj_fm(wb_, wc, ysT, 0, 256, ps2)
                        V(lambda ps1=ps1, cc=cc: v.tensor_tensor(out=m1[:, :], in0=ps1[:, 0:256], in1=gnT[:, cc, :], op=ALU.mult), [ps1.b, gnT.b], [m1.b])
                        V(lambda ps2=ps2, cc=cc: v.tensor_tensor(out=m2[:, :], in0=ps2[:, 0:256], in1=gsT[:, cc, :], op=ALU.mult), [ps2.b, gsT.b], [m2.b])
                        V(lambda cc=cc: v.tensor_tensor(out=mixT[:, cc, :], in0=m1[:, :], in1=m2[:, :], op=ALU.add), [m1.b, m2.b], [mixT.b])
                for ct in range(8):
                    w = wtile(wov, ct * 256, 256)
                    cs = slice(ct * 256, (ct + 1) * 256)
                    for tt in range(2):
                        ps = k.psum()
                        proj_tm(w, 256, mixT, tt, ps)
                        V(lambda ps=ps, cs=cs: v.tensor_tensor(out=tmp[:, :], in0=ps[:, 0:256], in1=m2bc[:, cs], op=ALU.mult), [ps.b, m2bc.b], [tmp.b])
                        V(lambda tt=tt, cs=cs: v.scalar_tensor_tensor(out=uu[:, tt, cs], in0=xo[:, tt, cs], scalar=DN_ALPHA, in1=tmp[:, :], op0=ALU.mult, op1=ALU.add),
                          [xo.b, tmp.b], [uu.b])
                for tt in range(2):
                    ln_stats(st, uu[:, tt, :], uu.b, mv, rstd)
                    V(lambda tt=tt: v.tensor_scalar(out=xn[:, :], in0=uu[:, tt, :], scalar1=mv[:, 0:1], scalar2=rstd[:, 0:1], op0=ALU.subtract, op1=ALU.mult),
                      [uu.b, mv.b, rstd.b], [xn.b])
                    V(lambda: v.tensor_tensor(out=xn[:, :], in0=xn[:, :], in1=lnbc[:, 0, :], op=ALU.mult), [xn.b, lnbc.b], [xn.b])
                    V(lambda: v.tensor_tensor(out=xn[:, :], in0=xn[:, :], in1=lnbc[:, 1, :], op=ALU.add), [xn.b, lnbc.b], [xn.b])
                    k.dma(k.sp, x1_d[row0 + tt * 128:row0 + (tt + 1) * 128, :], xn[:, :], reads=[xn.b])
                    if DBG:
                        k.dma(k.sp, dbg["x1"][row0 + tt * 128:row0 + (tt + 1) * 128, :], xn[:, :], reads=[xn.b])

        for u in range(NP if (LIM > 1 and LIM != 100) else 0):
            with contextlib.ExitStack() as es:
                unit(es, u, False)
                k.barrier()
        if SAMPLE and LIM > 1 and LIM != 100:
            with contextlib.ExitStack() as es:
                unit(es, 0, True)
                k.barrier()

        halves = [list(range(0, (NT + 1) // 2)), list(range((NT + 1) // 2, NT))]
        for half in halves:
            if not half or LIM <= 8:
                continue
            nt = len(half)
            NTOK = nt * 128
            with contextlib.ExitStack() as es:
                xT = sb(es, "xT", [128, 16, NTOK], BF16)
                hid = sb(es, "hid", [128, 16, NTOK], BF16)
                acc = sb(es, "acc", [128, nt, D])
                wg = [sb(es, "wg%d" % i, [128, 16, 256], BF16) for i in range(2)]
                wd = [sb(es, "wd%d" % i, [128, 16, 512], BF16) for i in range(2)]
                bgu = sb(es, "bgu", [128, NE, 32])
                k.dma(k.sp, bgu[:, :, :], bgu_d[:, :, :], writes=[bgu.b])
                V(lambda: v.memset(acc[:, :, :], 0.0), writes=[acc.b])
                with contextlib.ExitStack() as e2:
                    x1t = sb(e2, "x1t", [128, D])
                    xn = sb(e2, "xnB", [128, D])
                    x2T = sb(e2, "x2T", [128, 16, 128])
                    st = sb(e2, "stB", [128, 24])
                    mv = sb(e2, "mvB", [128, 2])
                    rstd = sb(e2, "rstdB", [128, 1])
                    wr = sb(e2, "wr", [128, 16, 32])
                    brb = sb(e2, "brb", [128, 32])
                    lg = sb(e2, "lg", [128, 32])
                    t8 = sb(e2, "t8", [128, 8])
                    msk = sb(e2, "msk", [128, 32])
                    sm = sb(e2, "sm", [128, 2])
                    k.dma(k.sp, wr[:, :, :], wr_d[:, :, :], writes=[wr.b])
                    k.dma(k.sp, brb[:, :], br_d[0:1, :].to_broadcast([128, 32]), writes=[brb.b])
                    x1b = Buf()
                    for i, tg in enumerate(half):
                        cv = 0 if tg < NPT else 1
                        k.dma(k.sp, x1t[:, :], x1_d[tg * 128:(tg + 1) * 128, :], writes=[x1t.b])
                        normalize_T(x1t[:, :], x1t.b, xn, st, mv, rstd, cv, 48, 64,
                                    [(lambda c: x2T[:, c, :], x2T.b)])
                        V(lambda i=i: v.tensor_copy(out=xT[:, :, i * 128:(i + 1) * 128], in_=x2T[:, :, :]), [x2T.b], [xT.b])
                        ps = k.psum()

                        def rt(ps=ps):
                            for kc in range(16):
                                r = pe.matmul(ps[:, 0:32], x2T[:, kc, :], wr[:, kc, :], start=(kc == 0), stop=(kc == 15))
                            return r
                        T(rt, [x2T.b, wr.b], [ps.b])
                        V(lambda ps=ps: v.tensor_tensor(out=lg[:, :], in0=ps[:, 0:32], in1=brb[:, :], op=ALU.add), [ps.b, brb.b], [lg.b])
                        V(lambda: v.max(out=t8[:, :], in_=lg[:, :]), [lg.b], [t8.b])
                        V(lambda: v.tensor_scalar(out=msk[:, :], in0=lg[:, :], scalar1=t8[:, 3:4], scalar2=None, op0=ALU.is_ge), [lg.b, t8.b], [msk.b])
                        V(lambda: v.tensor_scalar(out=sm[:, 0:1], in0=t8[:, 0:1], scalar1=-1.0, scalar2=None, op0=ALU.mult), [t8.b], [sm.b])
                        A(lambda: a.activation(out=lg[:, :], in_=lg[:, :], func=AF.Exp, bias=sm[:, 0:1], scale=1.0), [lg.b, sm.b], [lg.b])
                        V(lambda: v.tensor_tensor(out=lg[:, :], in0=lg[:, :], in1=msk[:, :], op=ALU.mult), [lg.b, msk.b], [lg.b])
                        V(lambda: v.reduce_sum(out=sm[:, 1:2], in_=lg[:, :], axis=AX.X), [lg.b], [sm.b])
                        V(lambda: v.reciprocal(out=sm[:, 1:2], in_=sm[:, 1:2]), [sm.b], [sm.b])
                        V(lambda tg=tg: v.tensor_scalar(out=comb[:, tg, :], in0=lg[:, :], scalar1=sm[:, 1:2], scalar2=None, op0=ALU.mult), [lg.b, sm.b], [comb.b])
                    k.barrier()
                with contextlib.ExitStack() as e2:
                    g1 = [sb(e2, "g1_%d" % i, [128, 512]) for i in range(2)]
                    sg = [sb(e2, "sg_%d" % i, [128, 512]) for i in range(2)]
                    u0 = [sb(e2, "u0_%d" % i, [128, 512]) for i in range(2)]
                    gi = [0]
                    tgs = [(t0, min(512, NTOK - t0)) for t0 in range(0, NTOK, 512)]
                    for e in range(NE):
                        for j in range(16):
                            w = wg[j % 2]
                            k.dma(k.pool, w[:, :, :], wgu_d[e, j].rearrange("p (kc c) -> p kc c", c=256), writes=[w.b], max_dma_last_dim=2048)
                            for (t0, n) in tgs:
                                psg, psu = k.psum(), k.psum()

                                def mm(ps, off, w=w, t0=t0, n=n):
                                    for kc in range(16):
                                        r = pe.matmul(ps[:, 0:n], w[:, kc, off:off + 128], xT[:, kc, t0:t0 + n], start=(kc == 0), stop=(kc == 15))
                                    return r
                                T(lambda psg=psg, mm=mm: mm(psg, 0), [w.b, xT.b], [psg.b])
                                T(lambda psu=psu, mm=mm: mm(psu, 128), [w.b, xT.b], [psu.b])
                                i2 = gi[0] % 2
                                gi[0] += 1
                                G1, SG, U0 = g1[i2], sg[i2], u0[i2]
                                V(lambda psg=psg, G1=G1, n=n, e=e, j=j: v.tensor_scalar(out=G1[:, 0:n], in0=psg[:, 0:n], scalar1=bgu[:, e, j:j + 1], scalar2=7.0,
                                                                                       op0=ALU.add, op1=ALU.min), [psg.b, bgu.b], [G1.b])
                                A(lambda G1=G1, SG=SG, n=n: a.activation(out=SG[:, 0:n], in_=G1[:, 0:n], func=AF.Sigmoid, scale=1.702), [G1.b], [SG.b])
                                A(lambda psu=psu, U0=U0, n=n, e=e, j=j: a.activation(out=U0[:, 0:n], in_=psu[:, 0:n], func=AF.Identity, bias=bgu[:, e, 16 + j:17 + j], scale=1.0),
                                  [psu.b, bgu.b], [U0.b])
                                V(lambda U0=U0, n=n: v.tensor_scalar(out=U0[:, 0:n], in0=U0[:, 0:n], scalar1=-7.0, scalar2=7.0, op0=ALU.max, op1=ALU.min), [U0.b], [U0.b])
                                V(lambda G1=G1, SG=SG, n=n: v.tensor_tensor(out=G1[:, 0:n], in0=G1[:, 0:n], in1=SG[:, 0:n], op=ALU.mult), [G1.b, SG.b], [G1.b])
                                V(lambda G1=G1, U0=U0, n=n, j=j, t0=t0: v.scalar_tensor_tensor(out=hid[:, j, t0:t0 + n], in0=U0[:, 0:n], scalar=1.0, in1=G1[:, 0:n],
                                                                                              op0=ALU.add, op1=ALU.mult), [U0.b, G1.b], [hid.b])
                        for ct in range(4):
                            w = wd[ct % 2]
                            k.dma(k.pool, w[:, :, :], wdn_d[e, ct].rearrange("p (j c) -> p j c", c=512), writes=[w.b], max_dma_last_dim=2048)
                            for i, tg in enumerate(half):
                                ps = k.psum()

                                def dn(ps=ps, w=w, i=i):
                                    for j in range(16):
                                        r = pe.matmul(ps[:, :], hid[:, j, i * 128:(i + 1) * 128], w[:, j, :], start=(j == 0), stop=(j == 15))
                                    return r
                                T(dn, [hid.b, w.b], [ps.b])
                                V(lambda ps=ps, i=i, tg=tg, ct=ct, e=e: v.scalar_tensor_tensor(out=acc[:, i, ct * 512:(ct + 1) * 512], in0=ps[:, :], scalar=comb[:, tg, e:e + 1],
                                                                                              in1=acc[:, i, ct * 512:(ct + 1) * 512], op0=ALU.mult, op1=ALU.add),
                                  [ps.b, comb.b, acc.b], [acc.b])
                    k.barrier()
                with contextlib.ExitStack() as e2:
                    bdn = sb(e2, "bdn", [32, D])
                    cT = sb(e2, "cT", [32, 128])
                    x1t = sb(e2, "x1f", [128, D])
                    m5bc = sb(e2, "m5bc", [128, 2, D])
                    lnbc = sb(e2, "ln2bc", [128, 2, D])
                    st = sb(e2, "stF", [128, 24])
                    mv = sb(e2, "mvF", [128, 2])
                    rstd = sb(e2, "rstdF", [128, 1])
                    if NE < 32:
                        V(lambda: v.memset(bdn[:, :], 0.0), writes=[bdn.b])
                    k.dma(k.sp, bdn[0:NE, :], bdn_d[:, :], writes=[bdn.b])
                    for cv in range(2):
                        k.dma(k.sp, m5bc[:, cv, :], mods_d[cv:cv + 1, 5 * D:6 * D].to_broadcast([128, D]), reads=[modsd_b], writes=[m5bc.b])
                        k.dma(k.sp, lnbc[:, cv, :], lnv_d[2 + cv:3 + cv, :].to_broadcast([128, D]), writes=[lnbc.b])
                    for i, tg in enumerate(half):
                        cv = 0 if tg < NPT else 1
                        ps = k.psum()
                        T(lambda ps=ps, tg=tg: pe.transpose(ps[0:32, 0:128], comb[:, tg, :], ident), [comb.b, cst.b], [ps.b])
                        V(lambda ps=ps: v.tensor_copy(out=cT[:, :], in_=ps[0:32, 0:128]), [ps.b], [cT.b])
                        k.dma(k.sp, x1t[:, :], x1_d[tg * 128:(tg + 1) * 128, :], writes=[x1t.b])
                        for ct in range(4):
                            cs = slice(ct * 512, (ct + 1) * 512)
                            ps = k.psum()
                            T(lambda ps=ps, cs=cs: pe.matmul(ps[:, :], cT[:, :], bdn[:, cs], start=True, stop=True), [cT.b, bdn.b], [ps.b])
                            V(lambda ps=ps, i=i, cs=cs: v.tensor_tensor(out=acc[:, i, cs], in0=acc[:, i, cs], in1=ps[:, :], op=ALU.add), [ps.b, acc.b], [acc.b])
                        V(lambda i=i, cv=cv: v.tensor_tensor(out=acc[:, i, :], in0=acc[:, i, :], in1=m5bc[:, cv, :], op=ALU.mult), [acc.b, m5bc.b], [acc.b])
                        V(lambda i=i: v.scalar_tensor_tensor(out=acc[:, i, :], in0=x1t[:, :], scalar=DN_ALPHA, in1=acc[:, i, :], op0=ALU.mult, op1=ALU.add), [x1t.b, acc.b], [acc.b])
                        ln_stats(st, acc[:, i, :], acc.b, mv, rstd)
                        V(lambda i=i: v.tensor_scalar(out=x1t[:, :], in0=acc[:, i, :], scalar1=mv[:, 0:1], scalar2=rstd[:, 0:1], op0=ALU.subtract, op1=ALU.mult),
                          [acc.b, mv.b, rstd.b], [x1t.b])
                        V(lambda: v.tensor_tensor(out=x1t[:, :], in0=x1t[:, :], in1=lnbc[:, 0, :], op=ALU.mult), [x1t.b, lnbc.b], [x1t.b])
                        V(lambda: v.tensor_tensor(out=x1t[:, :], in0=x1t[:, :], in1=lnbc[:, 1, :], op=ALU.add), [x1t.b, lnbc.b], [x1t.b])
                        if tg < NPT:
                            dst = yp_o[tg * 128:(tg + 1) * 128, :]
                        else:
                            dst = ys_o[(tg - NPT) * 128:(tg - NPT + 1) * 128, :]
                        k.dma(k.sp, dst, x1t[:, :], reads=[x1t.b])
                k.barrier()
        k.barrier()
    return nc


def _consts():
    c = np.zeros((128, 4, 128), np.float32)
    i = np.arange(128)
    c[:, 0, :] = np.eye(128, dtype=np.float32)
    c[:, 1, :] = (i[:, None] <= i[None, :]).astype(np.float32)
    c[:, 2, :] = (i[:, None] >= i[None, :]).astype(np.float32)
    c[:, 3, :] = 1.0
    return c


def _nbias(rpb, q):
    W, R, KR, KC = 64, 16, 8, 16
    col = np.arange(W)
    c0 = np.clip(col - KC // 2, 0, W - KC)
    colmask = (col[None, :] >= c0[:, None]) & (col[None, :] < c0[:, None] + KC)
    dc = np.clip(col[None, :] - col[:, None] + KC - 1, 0, 2 * KC - 2)
    out = np.full((16, 4, W, R, W), NEG, np.float32)
    for ri in range(4):
        r = 4 * q + ri
        r0 = int(np.clip(r - KR // 2, 0, R - KR))
        for kr in range(r0, r0 + KR):
            dr = kr - r + KR - 1
            blk = rpb[:, dr, :][:, dc]
            out[:, ri, :, kr, :] = np.where(colmask[None], blk, np.float32(NEG))
    return np.ascontiguousarray(out.reshape(16, 256, 1024))


def make_inputs(inp, core, NP=4, NE=32, SAMPLE=True, prompt_ids=None, sb=None, sq=None):
    f = lambda x: np.ascontiguousarray(x, dtype=np.float32)
    if prompt_ids is None:
        prompt_ids = list(range(core * NP, (core + 1) * NP))
    if sb is None:
        sb, sq = core // 4, core % 4
    m = {}
    m["xp"] = f(inp["x_prompt"][prompt_ids].reshape(NP * 256, D))
    m["xs_seq"] = f(inp["x_sample"][sb])
    m["xs_own"] = f(inp["x_sample"][sb, sq * 256:(sq + 1) * 256])
    cv = np.stack([inp["c_ctx"], inp["c"][sb]], axis=-1)
    m["cvT"] = f(cv.reshape(16, 128, 2).transpose(1, 0, 2))
    m["w_mod"] = f(inp["w_mod"][0])
    m["b_mod"] = f(inp["b_mod"][0][None])
    m["w_in"] = f(inp["w_in"][0])
    m["convw"] = f(inp["ssd_conv_w"][0].T.reshape(24, 128, 5).transpose(1, 0, 2))
    m["convb"] = f(inp["ssd_conv_b"][0].reshape(24, 128).T)
    m["dtb"] = f(inp["ssd_dt_bias"][0].reshape(1, 64))
    m["alog"] = f(inp["ssd_a_log"][0].reshape(1, 64))
    m["ssdd"] = f(inp["ssd_d"][0].reshape(1, 32))
    m["normw"] = f(inp["ssd_norm_w"][0].reshape(16, 128).T)
    m["w_ssd_out"] = f(inp["w_ssd_out"][0])
    m["w_na_out"] = f(inp["w_na_out"][0])
    m["w_o"] = f(inp["w_o"][0])
    m["lnv"] = f(np.stack([inp["ln1_g"][0], inp["ln1_b"][0], inp["ln2_g"][0], inp["ln2_b"][0]]))
    m["wr"] = f(inp["w_router"][0][:, :NE].reshape(16, 128, NE).transpose(1, 0, 2)) if NE == 32 else None
    if NE != 32:
        wr = np.zeros((128, 16, 32), np.float32)
        wr[:, :, :NE] = inp["w_router"][0][:, :NE].reshape(16, 128, NE).transpose(1, 0, 2)
        m["wr"] = wr
        br = np.full((1, 32), -1e4, np.float32)
        br[0, :NE] = inp["b_router"][0][:NE]
        m["br"] = br
    else:
        m["br"] = f(inp["b_router"][0][None])
    m["wgu"] = inp["_wgu"]
    m["bgu"] = inp["_bgu"]
    m["wdn"] = inp["_wdn"]
    m["bdn"] = f(inp["b_down"][0][:NE])
    m["cst"] = _consts()
    if SAMPLE:
        m["nbias"] = _nbias(np.asarray(inp["na_rpb"][0], np.float32), sq)
        m["kctxT"] = f(inp["cache_na_k"][sb, 0].transpose(1, 2, 0))
        m["vctx"] = f(inp["cache_na_v"][sb, 0].reshape(512, D))
        s0 = np.stack([inp["state_ssd_fwd"][sb, 0], inp["state_ssd_bwd"][sb, 0]])
        m["s0"] = f(s0.reshape(2, D, 128).transpose(0, 2, 1))
        sel = np.zeros((128, 16), np.float32)
        for sl in range(2):
            sel[:, (2 * sq + sl) * 2 + sl] = 1.0
        m["sel"] = sel
    return m


def prep_shared(inp, NE=32):
    wgu = np.asarray(inp["w_gate_up"][0][:NE], np.float32)
    w6 = wgu.reshape(NE, 16, 128, 2, 16, 128)
    inp["_wgu"] = np.ascontiguousarray(w6.transpose(0, 4, 2, 1, 3, 5)).reshape(NE, 16, 128, 16 * 256)
    bgu = np.asarray(inp["b_gate_up"][0][:NE], np.float32).reshape(NE, 32, 128)
    inp["_bgu"] = np.ascontiguousarray(bgu.transpose(2, 0, 1))
    wdn = np.asarray(inp["w_down"][0][:NE], np.float32).reshape(NE, 16, 128, 4, 512)
    inp["_wdn"] = np.ascontiguousarray(wdn.transpose(0, 3, 2, 1, 4)).reshape(NE, 4, 128, 16 * 512)


_NC_CACHE = {}


def kernel(**inputs):
    inp = {k_: np.asarray(v_) for k_, v_ in inputs.items()}
    prep_shared(inp)
    if "nc" not in _NC_CACHE:
        _NC_CACHE["nc"] = build()
    nc = _NC_CACHE["nc"]
    in_maps = [make_inputs(inp, c) for c in range(NCORES)]
    res = run_bass_kernel_spmd(nc, in_maps, core_ids=list(range(NCORES)))
    r = res.results
    y_p = np.concatenate([r[c]["y_p"].reshape(4, 256, D) for c in range(NCORES)], axis=0)
    y_s = np.stack([np.concatenate([r[b * 4 + q]["y_s"] for q in range(4)], axis=0) for b in range(2)], axis=0)
    nk = np.concatenate([r[c]["nk"].reshape(4, 1, 256, 16, 128) for c in range(NCORES)], axis=0)
    nv = np.concatenate([r[c]["nv"].reshape(4, 1, 256, 16, 128) for c in range(NCORES)], axis=0)
    hf = np.concatenate([r[c]["hf"].reshape(4, 1, 32, 64, 128) for c in range(NCORES)], axis=0)
    hb = np.concatenate([r[c]["hb"].reshape(4, 1, 32, 64, 128) for c in range(NCORES)], axis=0)
    return (y_p.astype(np.float32), y_s.astype(np.float32), nk.astype(np.float32), nv.astype(np.float32),
            hf.astype(np.float32), hb.astype(np.float32))
```
